# Optimizing a Trainium2 kernel written in Bass

```python
import math
import jax, jax.numpy as jnp
from jax import lax
import numpy as np

D_MODEL = 2048
BATCH = 1
SEQ = 8192
DEPTH = 1

N_ATTN_HEADS = 16
HEAD_DIM = 64
ATTN_WIDTH = N_ATTN_HEADS * HEAD_DIM
CONV_CHANNELS = D_MODEL - ATTN_WIDTH
CONV_WIDTH = 31
MIX_WIDTH = CONV_CHANNELS + ATTN_WIDTH
IN_WIDTH = 2 * CONV_CHANNELS + 3 * ATTN_WIDTH
DILATED_PATTERNS = ((128, 1), (512, 4), (2048, 16))
BLOCK = 128
NUM_BUCKETS = 32
MAX_DISTANCE = 2048
N_GROUPS = 4
EXPERTS_PER_GROUP = 8
EXPERT_TOP_K = 2
D_FF_EXPERT = D_MODEL // 2
NORM_EPS = 1e-6
NEG_INF = -1e30

kernel_name = "hymba_conformer_dilated_hmoe_layer"


def _rmsnorm(x, g):
    xf = x.astype(jnp.float32)
    y = xf * lax.rsqrt(jnp.mean(xf * xf, axis=-1, keepdims=True) + NORM_EPS)
    return (y * g.astype(jnp.float32)).astype(x.dtype)


def _layernorm(x, g, b):
    xf = x.astype(jnp.float32)
    mu = jnp.mean(xf, axis=-1, keepdims=True)
    var = jnp.mean(jnp.square(xf - mu), axis=-1, keepdims=True)
    y = (xf - mu) * lax.rsqrt(var + NORM_EPS)
    return (y * g.astype(jnp.float32) + b.astype(jnp.float32)).astype(x.dtype)


def _t5_bucket(dist):
    max_exact = NUM_BUCKETS // 2
    nf = jnp.maximum(dist, 1).astype(jnp.float32)
    large = max_exact + (jnp.log(nf / max_exact) / math.log(MAX_DISTANCE / max_exact)
                         * (NUM_BUCKETS - max_exact)).astype(jnp.int32)
    large = jnp.minimum(large, NUM_BUCKETS - 1)
    return jnp.where(dist < max_exact, dist, large)


def _conv_mixer(a_val, a_gate, conv_w, conv_b, ln_g, ln_b):
    u = a_val * jax.nn.sigmoid(a_gate)
    kern = conv_w.astype(u.dtype)[:, None, :]
    u = lax.conv_general_dilated(u, kern, window_strides=(1,), padding=[(CONV_WIDTH - 1, 0)],
                                 dimension_numbers=("NWC", "WIO", "NWC"),
                                 feature_group_count=CONV_CHANNELS) + conv_b.astype(u.dtype)
    return jax.nn.silu(_layernorm(u, ln_g, ln_b))


def _dilated_window_attention(q, k, v, rel_bias, window, dilation):
    B, H, S, hd = q.shape
    span = window // dilation
    assert span <= BLOCK
    chunk = dilation * BLOCK
    s_pad = -(-S // chunk) * chunk
    L = s_pad // dilation
    nb = L // BLOCK
    padw = ((0, 0), (0, 0), (0, s_pad - S), (0, 0))

    def to_blocks(t):
        t = jnp.pad(t, padw).reshape(B, H, L, dilation, hd).transpose(0, 1, 3, 2, 4)
        return t.reshape(B, H, dilation, nb, BLOCK, hd)

    def with_prev(t):
        prev = jnp.pad(t[:, :, :, :-1], ((0, 0), (0, 0), (0, 0), (1, 0), (0, 0), (0, 0)))
        return jnp.concatenate([prev, t], axis=4)

    qb = to_blocks(q)
    kc = with_prev(to_blocks(k))
    vc = with_prev(to_blocks(v))

    qi = jnp.arange(BLOCK)[:, None]
    kj = jnp.arange(2 * BLOCK)[None, :]
    rel = BLOCK + qi - kj
    in_band = (rel >= 0) & (rel <= span)
    key_idx = jnp.arange(nb)[:, None, None] * BLOCK + kj[None] - BLOCK
    mask = in_band[None] & (key_idx >= 0)
    bucket = _t5_bucket(jnp.clip(rel, 0, span) * dilation)
    bias = jnp.transpose(rel_bias.astype(jnp.float32)[bucket], (2, 0, 1))

    scale = 1.0 / math.sqrt(hd)
    scores = jnp.einsum("bhrnqd,bhrnkd->bhrnqk", qb, kc) * scale + bias[None, :, None, None]
    scores = jnp.where(mask[None, None, None], scores, NEG_INF)
    m = jnp.max(scores, axis=-1, keepdims=True)
    p = jnp.exp(scores - m)
    denom = jnp.sum(p, axis=-1)
    o = jnp.einsum("bhrnqk,bhrnkd->bhrnqd", p, vc) / denom[..., None]
    lse = m[..., 0] + jnp.log(denom)

    o = o.reshape(B, H, dilation, L, hd).transpose(0, 1, 3, 2, 4).reshape(B, H, s_pad, hd)[:, :, :S]
    lse = lse.reshape(B, H, dilation, L).transpose(0, 1, 3, 2).reshape(B, H, s_pad)[:, :, :S]
    return o, lse


def _dilated_attention_mixer(q, k, v, q_g, k_g, rel_bias):
    B, S, _ = q.shape
    qh = _rmsnorm(q.reshape(B, S, N_ATTN_HEADS, HEAD_DIM), q_g).astype(jnp.float32).transpose(0, 2, 1, 3)
    kh = _rmsnorm(k.reshape(B, S, N_ATTN_HEADS, HEAD_DIM), k_g).astype(jnp.float32).transpose(0, 2, 1, 3)
    vh = v.reshape(B, S, N_ATTN_HEADS, HEAD_DIM).astype(jnp.float32).transpose(0, 2, 1, 3)
    outs, lses = [], []
    for window, dilation in DILATED_PATTERNS:
        o, lse = _dilated_window_attention(qh, kh, vh, rel_bias, window, dilation)
        outs.append(o)
        lses.append(lse)
    o = jnp.stack(outs)
    wts = jax.nn.softmax(jnp.stack(lses), axis=0)
    o = jnp.sum(wts[..., None] * o, axis=0)
    return o.transpose(0, 2, 1, 3).reshape(B, S, ATTN_WIDTH).astype(q.dtype)


def _hier_moe(xn, w_rg, b_rg, w_re, b_re, w_gate, w_up, w_down):
    B, S, D = xn.shape
    t = xn.reshape(B * S, D)
    glog = (t @ w_rg + b_rg).astype(jnp.float32)
    gprob = jax.nn.softmax(glog, axis=-1)
    _, gidx = lax.top_k(glog, 1)
    gw = jnp.take_along_axis(gprob, gidx, axis=1)[:, 0]
    elog = (jnp.einsum("td,gde->tge", t, w_re) + b_re).astype(jnp.float32)
    elog_sel = jnp.take_along_axis(elog, gidx[:, :, None], axis=1)[:, 0]
    top_v, top_i = lax.top_k(elog_sel, EXPERT_TOP_K)
    top_w = jax.nn.softmax(top_v, axis=-1)
    ew = jnp.sum(jax.nn.one_hot(top_i, EXPERTS_PER_GROUP, dtype=jnp.float32) * top_w[..., None], axis=1)
    gate = (jax.nn.one_hot(gidx[:, 0], N_GROUPS, dtype=jnp.float32)[:, :, None]
            * (gw[:, None] * ew)[:, None, :]).astype(t.dtype)
    y = jnp.zeros_like(t)
    for g in range(N_GROUPS):
        h = (jax.nn.silu(jnp.einsum("td,edf->tef", t, w_gate[g]))
             * jnp.einsum("td,edf->tef", t, w_up[g]) * gate[:, g, :, None])
        y = y + jnp.einsum("tef,efd->td", h, w_down[g])
    return y.reshape(B, S, D)


def setup_inputs(seed: int = 0) -> dict:
    key = jax.random.key(seed)
    ks = jax.random.split(key, 20)
    f32 = jnp.float32
    nrm = lambda k, shape, s: jax.random.normal(k, shape, f32) * s
    return {
        "x": nrm(ks[0], (BATCH, SEQ, D_MODEL), 1.0),
        "norm1_g": 1.0 + nrm(ks[1], (DEPTH, D_MODEL), 0.02),
        "w_in": nrm(ks[2], (DEPTH, D_MODEL, IN_WIDTH), D_MODEL ** -0.5),
        "q_norm_g": 1.0 + nrm(ks[3], (DEPTH, HEAD_DIM), 0.02),
        "k_norm_g": 1.0 + nrm(ks[4], (DEPTH, HEAD_DIM), 0.02),
        "conv_w": nrm(ks[5], (DEPTH, CONV_WIDTH, CONV_CHANNELS), CONV_WIDTH ** -0.5),
        "conv_b": nrm(ks[6], (DEPTH, CONV_CHANNELS), 0.02),
        "conv_ln_g": 1.0 + nrm(ks[7], (DEPTH, CONV_CHANNELS), 0.02),
        "conv_ln_b": nrm(ks[8], (DEPTH, CONV_CHANNELS), 0.02),
        "rel_bias": nrm(ks[9], (NUM_BUCKETS, N_ATTN_HEADS), 0.1),
        "w_out": nrm(ks[10], (DEPTH, MIX_WIDTH, D_MODEL), MIX_WIDTH ** -0.5),
        "norm2_g": 1.0 + nrm(ks[11], (DEPTH, D_MODEL), 0.02),
        "w_router_group": nrm(ks[12], (DEPTH, D_MODEL, N_GROUPS), D_MODEL ** -0.5),
        "b_router_group": nrm(ks[13], (DEPTH, N_GROUPS), 0.01),
        "w_router_expert": nrm(ks[14], (DEPTH, N_GROUPS, D_MODEL, EXPERTS_PER_GROUP), D_MODEL ** -0.5),
        "b_router_expert": nrm(ks[15], (DEPTH, N_GROUPS, EXPERTS_PER_GROUP), 0.01),
        "w_gate": nrm(ks[16], (DEPTH, N_GROUPS, EXPERTS_PER_GROUP, D_MODEL, D_FF_EXPERT), D_MODEL ** -0.5),
        "w_up": nrm(ks[17], (DEPTH, N_GROUPS, EXPERTS_PER_GROUP, D_MODEL, D_FF_EXPERT), D_MODEL ** -0.5),
        "w_down": nrm(ks[18], (DEPTH, N_GROUPS, EXPERTS_PER_GROUP, D_FF_EXPERT, D_MODEL), D_FF_EXPERT ** -0.5),
    }


def reference(x, norm1_g, w_in, q_norm_g, k_norm_g, conv_w, conv_b, conv_ln_g, conv_ln_b,
              rel_bias, w_out, norm2_g, w_router_group, b_router_group, w_router_expert,
              b_router_expert, w_gate, w_up, w_down):
    C, A = CONV_CHANNELS, ATTN_WIDTH
    for layer in range(DEPTH):
        xn = _rmsnorm(x, norm1_g[layer])
        proj = xn @ w_in[layer]
        a_val, a_gate, q, k, v = jnp.split(proj, [C, 2 * C, 2 * C + A, 2 * C + 2 * A], axis=-1)
        conv_out = _conv_mixer(a_val, a_gate, conv_w[layer], conv_b[layer],
                               conv_ln_g[layer], conv_ln_b[layer])
        attn_out = _dilated_attention_mixer(q, k, v, q_norm_g[layer], k_norm_g[layer], rel_bias)
        x = x + jnp.concatenate([conv_out, attn_out], axis=-1) @ w_out[layer]
        hn = _rmsnorm(x, norm2_g[layer])
        x = x + _hier_moe(hn, w_router_group[layer], b_router_group[layer], w_router_expert[layer],
                          b_router_expert[layer], w_gate[layer], w_up[layer], w_down[layer])
    return x
```

```python
import math
from contextlib import ExitStack
import numpy as np
import concourse.bass as bass
import concourse.mybir as mybir
from concourse.bass_utils import run_bass_kernel_spmd

F32 = mybir.dt.float32
BF16 = mybir.dt.bfloat16
I32 = mybir.dt.int32
ALU = mybir.AluOpType
AF = mybir.ActivationFunctionType
AX = mybir.AxisListType

ENGS = ("sync", "act", "pool", "pe", "dve")
NCORES = 8
SEQ = 8192
D = 2048
TOK = SEQ // NCORES
HALO = 2048
NT = HALO + TOK
NE = 32
CAP = 128
EPS = 1e-6
NEG = -30000.0
ZROW = NE * CAP
STOP_AFTER = None
NCV = 12


class Res:
    __slots__ = ("name", "writer", "readers", "dsem", "dcount")

    def __init__(self, name, dsem=None):
        self.name = name
        self.writer = None
        self.readers = {}
        self.dsem = dsem
        self.dcount = 0


class Sched:
    def __init__(self, nc, stack):
        self.nc = nc
        self.stack = stack
        self.q = {e: [] for e in ENGS}
        self.cnt = {e: 0 for e in ENGS}
        self.seen = {e: {} for e in ENGS}
        self.sem = {e: stack.enter_context(nc.semaphore("s_" + e)) for e in ENGS}
        self.final_toks = []
        self.dsems = []

    def dres(self, name):
        s = self.stack.enter_context(self.nc.semaphore("d_" + name))
        r = Res(name, dsem=s)
        self.dsems.append(r)
        return r

    def op(self, eng, fn, reads=(), writes=(), dma=None, gates=()):
        need = {}

        def add(tok, war=False):
            if tok is None:
                return
            sem, val, teng = tok
            if teng == eng and teng == "pe":
                return
            if war and teng == eng:
                return
            k = id(sem)
            if k not in need or need[k][1] < val:
                need[k] = (sem, val)

        for r in reads:
            add(r.writer)
        for g in gates:
            add(g)
        for w in writes:
            add(w.writer)
            for tok in w.readers.values():
                add(tok, war=True)
        waits = []
        seen = self.seen[eng]
        for k, (sem, val) in need.items():
            if seen.get(k, 0) >= val:
                continue
            seen[k] = val
            waits.append((sem, val))
        if dma is None:
            self.cnt[eng] += 1
            tok = (self.sem[eng], self.cnt[eng], eng)
            inc = (self.sem[eng], 1)
        else:
            dma.dcount += 16
            tok = (dma.dsem, dma.dcount, "dma")
            inc = (dma.dsem, 16)
        self.q[eng].append((waits, fn, inc))
        for r in reads:
            k = id(tok[0])
            old = r.readers.get(k)
            if old is None or old[1] < tok[1]:
                r.readers[k] = tok
        for w in writes:
            w.writer = tok
            w.readers = {}
        return tok

    def barrier(self):
        toks = [(self.sem[e], self.cnt[e], e) for e in ENGS if self.cnt[e] > 0]
        toks += [(r.dsem, r.dcount, "dma") for r in self.dsems if r.dcount > 0]
        for eng in ENGS:
            waits = []
            seen = self.seen[eng]
            for sem, val, teng in toks:
                if teng == eng:
                    continue
                k = id(sem)
                if seen.get(k, 0) >= val:
                    continue
                seen[k] = val
                waits.append((sem, val))
            if waits:
                self.q[eng].append((waits, None, None))

    def finish(self, toks):
        self.final_toks = list(toks)

    def replay(self):
        nc = self.nc
        q = self.q
        final = self.final_toks

        def run(engobj, items):
            for waits, fn, inc in items:
                for sem, val in waits:
                    engobj.wait_ge(sem, val)
                if fn is None:
                    continue
                ins = fn(engobj)
                ins.then_inc(inc[0], inc[1])

        with nc.Block() as block:
            @block.sync
            def _(e):
                run(e, q["sync"])
                for sem, val, _t in final:
                    e.wait_ge(sem, val)

            @block.scalar
            def _(e):
                run(e, q["act"])

            @block.gpsimd
            def _(e):
                run(e, q["pool"])

            @block.tensor
            def _(e):
                run(e, q["pe"])

            @block.vector
            def _(e):
                run(e, q["dve"])


def _t5_bucket_np(dist):
    max_exact = 16
    nf = np.maximum(dist, 1).astype(np.float32)
    large = max_exact + (np.log(nf / np.float32(max_exact)) / np.float32(math.log(2048 / max_exact))
                         * np.float32(32 - max_exact)).astype(np.int32)
    large = np.minimum(large, 31)
    return np.where(dist < max_exact, dist, large)


def _bucket_tables():
    kk = np.arange(128)[:, None]
    bidx = np.zeros((128, 1536), np.int64)
    mask = np.zeros((128, 1536), bool)
    for pi, d in enumerate((1, 4)):
        q = np.arange(128)[None, :]
        relA = 128 + q - kk
        relB = q - kk
        for rep in range(2):
            c0 = pi * 512 + rep * 256
            bidx[:, c0:c0 + 128] = _t5_bucket_np(np.clip(relA, 0, 128) * d)
            mask[:, c0:c0 + 128] = (relA >= 0) & (relA <= 128)
            bidx[:, c0 + 128:c0 + 256] = _t5_bucket_np(np.clip(relB, 0, 128) * d)
            mask[:, c0 + 128:c0 + 256] = (relB >= 0) & (relB <= 128)
    q = np.arange(64)[None, :]
    relA = 128 + q - kk
    relB = q + 64 - kk
    for rep in range(4):
        c0 = 1024 + rep * 128
        bidx[:, c0:c0 + 64] = _t5_bucket_np(np.clip(relA, 0, 128) * 16)
        mask[:, c0:c0 + 64] = (relA >= 0) & (relA <= 128)
        bidx[:, c0 + 64:c0 + 128] = _t5_bucket_np(np.clip(relB, 0, 128) * 16)
        mask[:, c0 + 64:c0 + 128] = (relB >= 0) & (relB <= 128) & (kk >= 64)
    return bidx, mask


def _key_tiles():
    tiles = []
    for i in range(9):
        tiles.append((1920 + 128 * i, 1))
    for r in range(4):
        for i in range(3):
            tiles.append((2048 - 512 + r + 512 * i, 4))
    for r in range(16):
        tiles.append((r, 16))
        tiles.append((1024 + r, 16))
    return tiles


KT = _key_tiles()
NKT = len(KT)


def build_program():
    nc = bass.Bass("TRN2", target_bir_lowering=False)
    dram = lambda name, shape, dt, kind="ExternalInput": nc.dram_tensor(name, list(shape), dt, kind=kind).ap()
    xh = dram("xh", [NT, D], F32)
    w_in = dram("w_in", [D, 5120], F32)
    w_out = dram("w_out", [D, D], F32)
    pst = dram("pst", [48, 128], F32)
    g1 = dram("g1", [1, D], F32)
    g2 = dram("g2", [1, D], F32)
    convw = dram("convw", [31, 1024], F32)
    bm_d = dram("bm", [16, 128, 1536], F32)
    vcol_d = dram("vcol", [128, NKT], F32)
    wr_d = dram("wr", [D, 36], F32)
    br_d = dram("br", [1, 36], F32)
    wg_d = dram("wg", [NE, D, 1024], F32)
    wu_d = dram("wu", [NE, D, 1024], F32)
    wd_d = dram("wd", [NE, 1024, D], F32)
    out_d = dram("out", [TOK, D], F32, kind="ExternalOutput")
    yd = dram("yd", [NE * CAP + 128, D], F32, kind="Internal")
    wgc = dram("wgc", [NE, D, 1024], BF16, kind="Internal")
    wdc = dram("wdc", [NCV, 1024, D], BF16, kind="Internal")

    with ExitStack() as st:
        S = Sched(nc, st)

        ARENA_BYTES = 212736
        arena = st.enter_context(nc.sbuf_tensor("arena", [128, ARENA_BYTES // 2], BF16))
        ar = {"lo": 0, "hi": ARENA_BYTES}

        def _view(off, shape, dt):
            esz = 2 if dt == BF16 else 4
            nel = 1
            for d_ in shape[1:]:
                nel *= d_
            v = arena[0:shape[0], off // 2:(off + nel * esz) // 2]
            if dt != BF16:
                v = v.bitcast(dt)
            if len(shape) == 3:
                v = v.rearrange("p (a b) -> p a b", a=shape[1])
            elif len(shape) == 4:
                v = v.rearrange("p (a b c) -> p a b c", a=shape[1], b=shape[2])
            return v

        def _nbytes(shape, dt):
            nel = 1
            for d_ in shape[1:]:
                nel *= d_
            return ((nel * (2 if dt == BF16 else 4)) + 63) // 64 * 64

        class Scope:
            def __enter__(self):
                self.mark = ar["lo"]
                return self

            def __exit__(self, *a):
                ar["lo"] = self.mark
                return False

        def sb(stack, name, shape, dt, at=None):
            nb = _nbytes(shape, dt)
            if at is not None:
                return _view(at, shape, dt)
            if stack is st:
                ar["hi"] -= nb
                off = ar["hi"]
            else:
                off = ar["lo"]
                ar["lo"] += nb
            assert ar["lo"] <= ar["hi"], ("SBUF arena overflow at", name, ar)
            ar["last"] = off
            ar["peak"] = max(ar.get("peak", 0), ar["lo"] + ARENA_BYTES - ar["hi"])
            return _view(off, shape, dt)

        ps = st.enter_context(nc.psum_tensor("ps", [128, 4096], F32))
        psb = ps[:].bitcast(BF16)
        rP = [Res("bank%d" % i) for i in range(8)]
        bk = lambda b, a=0, n=512: ps[:, b * 512 + a: b * 512 + a + n]
        ssl = lambda s0, n, stp: slice(s0, s0 + (n - 1) * stp + 1, stp)

        def MM(out, lhsT, rhs, start, stop, reads, writes):
            S.op("pe", lambda e: e.matmul(out, lhsT=lhsT, rhs=rhs, start=start, stop=stop,
                                          skip_group_check=True), reads, writes)

        def TR(out, in_, ident, reads, writes):
            S.op("pe", lambda e: e.transpose(out, in_, ident), reads, writes)

        def ACT(out, in_, func, reads, writes, scale=1.0, bias=None, accum_out=None):
            kw = {}
            if bias is not None:
                kw["bias"] = bias
            if accum_out is not None:
                kw["accum_out"] = accum_out
            S.op("act", lambda e: e.activation(out=out, in_=in_, func=func, scale=scale, **kw), reads, writes)

        def TT(out, in0, in1, op, reads, writes, eng="dve"):
            S.op(eng, lambda e: e.tensor_tensor(out=out, in0=in0, in1=in1, op=op), reads, writes)

        def TS(out, in0, s1, s2, op0, op1, reads, writes, eng="dve", accum_out=None):
            if accum_out is None:
                S.op(eng, lambda e: e.tensor_scalar(out=out, in0=in0, scalar1=s1, scalar2=s2, op0=op0, op1=op1),
                     reads, writes)
            else:
                S.op(eng, lambda e: e.tensor_scalar(out=out, in0=in0, scalar1=s1, scalar2=s2, op0=op0, op1=op1,
                                                    accum_out=accum_out), reads, writes)

        def STT(out, in0, scalar, in1, op0, op1, reads, writes, eng="dve"):
            S.op(eng, lambda e: e.scalar_tensor_tensor(out=out, in0=in0, scalar=scalar, in1=in1, op0=op0, op1=op1),
                 reads, writes)

        def CP(out, in_, reads, writes, eng="dve"):
            if eng == "act":
                S.op("act", lambda e: e.copy(out=out, in_=in_), reads, writes)
            else:
                S.op(eng, lambda e: e.tensor_copy(out=out, in_=in_), reads, writes)

        def DMA(eng, out, in_, reads, writes, dres, gates=()):
            return S.op(eng, lambda e: e.dma_start(out=out, in_=in_), reads, writes, dma=dres, gates=gates)

        r_cv = S.dres("cv")
        cv_list = []
        for ex_ in range(NE):
            todo = [(wg_d, wgc, D)] + ([(wd_d, wdc, 1024)] if ex_ < NCV else [])
            for (src, dst, rows) in todo:
                hr = rows // 2
                for hh in range(2):
                    cv_list.append((dst[ex_, hh * hr:(hh + 1) * hr, :], src[ex_, hh * hr:(hh + 1) * hr, :]))
        cv_pos = [0]

        def emit_cv(n, gates=()):
            for _ in range(n):
                if cv_pos[0] >= len(cv_list):
                    return
                o_, i_ = cv_list[cv_pos[0]]
                cv_pos[0] += 1
                DMA("pool", o_, i_, (), [], r_cv, gates=gates)

        identb = sb(st, "identb", [128, 128], BF16)
        identf = sb(st, "identf", [128, 128], F32)
        ustrict = sb(st, "ustrict", [128, 128], BF16)
        onesb = sb(st, "onesb", [128, 128], BF16)
        onesf = sb(st, "onesf", [128, 128], F32)
        blockones = sb(st, "blockones", [128, 128], BF16)
        iota_row = sb(st, "iota_row", [128, 128], F32)
        ebase = sb(st, "ebase", [128, 32], F32)
        tmpc = sb(st, "tmpc", [128, 128], F32, at=0)
        tmpc2 = sb(st, "tmpc2", [128, 128], F32, at=512)
        epsb = sb(st, "epsb", [128, 1], F32)
        r_const = Res("const")
        r_tmpc = Res("tmpc")
        r_tmpc2 = Res("tmpc2")
        S.op("pool", lambda e: e.iota(tmpc[:], pattern=[[1, 128]], base=0, channel_multiplier=-1,
                                      allow_small_or_imprecise_dtypes=True), (), [r_tmpc])
        S.op("dve", lambda e: e.tensor_single_scalar(out=identb[:], in_=tmpc[:], scalar=0.0, op=ALU.is_equal),
             [r_tmpc], [r_const])
        S.op("dve", lambda e: e.tensor_single_scalar(out=identf[:], in_=tmpc[:], scalar=0.0, op=ALU.is_equal),
             [r_tmpc], [r_const])
        S.op("dve", lambda e: e.tensor_single_scalar(out=ustrict[:], in_=tmpc[:], scalar=0.0, op=ALU.is_gt),
             [r_tmpc], [r_const])
        S.op("dve", lambda e: e.memset(onesb[:], 1.0), (), [r_const])
        S.op("dve", lambda e: e.memset(onesf[:], 1.0), (), [r_const])
        S.op("dve", lambda e: e.memset(epsb[:], EPS), (), [r_const])
        S.op("pool", lambda e: e.iota(iota_row[:], pattern=[[1, 128]], base=0, channel_multiplier=0,
                                      allow_small_or_imprecise_dtypes=True), (), [r_const])
        S.op("pool", lambda e: e.iota(tmpc2[:], pattern=[[0, 128]], base=0, channel_multiplier=1,
                                      allow_small_or_imprecise_dtypes=True), (), [r_tmpc2])
        S.op("dve", lambda e: e.tensor_single_scalar(out=tmpc2[:], in_=tmpc2[:], scalar=64.0, op=ALU.is_ge),
             [r_tmpc2], [r_tmpc2])
        S.op("dve", lambda e: e.tensor_single_scalar(out=tmpc[:], in_=iota_row[:], scalar=64.0, op=ALU.is_ge),
             [r_const, r_tmpc], [r_tmpc])
        TT(blockones[:], tmpc[:], tmpc2[:], ALU.is_equal, [r_tmpc, r_tmpc2], [r_const])
        S.op("pool", lambda e: e.iota(ebase[:], pattern=[[CAP, 32]], base=0, channel_multiplier=0,
                                      allow_small_or_imprecise_dtypes=True), (), [r_const])

        pstage = sb(st, "pstage", [48, 128], F32, at=1024)
        prm = sb(st, "prm", [128, 48], F32)
        cwst = sb(st, "cwst", [31, 1024], F32, at=1536)
        cw = sb(st, "cw", [128, 8, 31], F32)
        r_pstage = S.dres("pstage")
        r_cwst = S.dres("cwst")
        r_prm = Res("prm")
        r_cw = Res("cw")
        DMA("sync", pstage[:], pst, (), [r_pstage], r_pstage)
        DMA("sync", cwst[:], convw, (), [r_cwst], r_cwst)
        TR(bk(0, 0, 48), pstage[:], identf[0:48, 0:48], [r_pstage, r_const], [rP[0]])
        CP(prm[:], bk(0, 0, 48), [rP[0]], [r_prm])
        for j in range(8):
            TR(bk(1, j * 32, 31), cwst[:, j * 128:(j + 1) * 128], identf[0:31, 0:31], [r_cwst, r_const], [rP[1]])
        CP(cw[:], bk(1, 0, 256).rearrange("p (j k) -> p j k", j=8)[:, :, 0:31], [rP[1]], [r_cw])

        convT = sb(st, "convT", [128, 8, TOK], BF16)
        attnT = sb(st, "attnT", [128, 8, TOK], BF16)
        attn_off = ar["last"]
        r_convT = [Res("convT%d" % j) for j in range(8)]
        r_attnT = [Res("attnT%d" % j) for j in range(8)]

        S.barrier()
        with Scope() as s1:
            xnT = sb(s1, "xnT", [128, 16, NT], BF16)
            r_xnT = [Res("xnT%d" % t) for t in range(24)]
            wbuf = [sb(s1, "wbuf%d" % i, [128, 16, 128], BF16) for i in range(4)]
            r_wbuf = [S.dres("wbuf%d" % i) for i in range(4)]
            w_in_v = w_in.rearrange("(kc p) n -> p kc n", p=128)
            wslot = [0]

            def load_w(col0):
                i = wslot[0] % 4
                wslot[0] += 1
                DMA("pool", wbuf[i][:], w_in_v[:, :, col0:col0 + 128], (), [r_wbuf[i]], r_wbuf[i])
                return i

            with Scope() as sa:
                xt = [sb(sa, "xt%d" % i, [128, D], F32) for i in range(2)]
                r_xt = [S.dres("xt%d" % i) for i in range(2)]
                xn = [sb(sa, "xn%d" % i, [128, D], BF16) for i in range(2)]
                r_xn = [Res("xn%d" % i) for i in range(2)]
                g1bc = sb(sa, "g1bc", [128, D], F32)
                r_g1 = S.dres("g1bc")
                junk = sb(sa, "junk", [128, D], BF16)
                r_junk = Res("junk")
                ssq = sb(sa, "ssq", [128, 24], F32)
                rstd = sb(sa, "rstd", [128, 24], F32)
                r_ss = [Res("ss%d" % t) for t in range(24)]
                DMA("sync", g1bc[:], g1.broadcast_to([128, D]), (), [r_g1], r_g1)
                for tt in range(24):
                    b = tt % 2
                    tx = DMA("sync", xt[b][:], xh[tt * 128:(tt + 1) * 128, :], (), [r_xt[b]], r_xt[b])
                    if tt % 4 == 3:
                        emit_cv(1, [tx])
                    ACT(junk[:], xt[b][:], AF.Square, [r_xt[b]], [r_junk, r_ss[tt]], accum_out=ssq[:, tt:tt + 1])
                    ACT(rstd[:, tt:tt + 1], ssq[:, tt:tt + 1], AF.Ln, [r_ss[tt], r_const], [r_ss[tt]],
                        scale=1.0 / D, bias=epsb[:, 0:1])
                    ACT(rstd[:, tt:tt + 1], rstd[:, tt:tt + 1], AF.Exp, [r_ss[tt]], [r_ss[tt]], scale=-0.5)
                    STT(xn[b][:], xt[b][:], rstd[:, tt:tt + 1], g1bc[:], ALU.mult, ALU.mult,
                        [r_xt[b], r_ss[tt], r_g1], [r_xn[b]])
                    for half in range(2):
                        bank = (2 * tt + half) % 4
                        for j in range(8):
                            kc = half * 8 + j
                            TR(psb[:, bank * 1024 + j * 128: bank * 1024 + (j + 1) * 128],
                               xn[b][:, kc * 128:(kc + 1) * 128], identb[:], [r_xn[b], r_const], [rP[bank]])
                        CP(xnT[:, half * 8:(half + 1) * 8, tt * 128:(tt + 1) * 128],
                           psb[:, bank * 1024:(bank + 1) * 1024].rearrange("p (j t) -> p j t", j=8),
                           [rP[bank]], [r_xnT[tt]], eng=("act" if half == 0 else "dve"))
                S.barrier()

            with Scope() as sc:
                NU = 1152
                ub = [sb(sc, "u%d" % i, [128, NU], F32) for i in range(2)]
                r_u = [Res("u%d" % i) for i in range(2)]
                sig = [sb(sc, "sig%d" % i, [128, 512], F32) for i in range(2)]
                r_sig = [Res("sig%d" % i) for i in range(2)]
                craw = sb(sc, "craw", [128, 8, TOK], F32)
                r_craw = [Res("craw%d" % j) for j in range(8)]
                csq = sb(sc, "csq", [128, 512], F32)
                r_csq = Res("csq")
                mean = sb(sc, "mean", [128, TOK], F32, at=attn_off)
                lrs = sb(sc, "lrs", [128, TOK], F32, at=attn_off + 4096)
                r_mean = Res("mean")
                r_lrs = Res("lrs")
                tln = [sb(sc, "tln%d" % i, [128, TOK], F32, at=attn_off + 8192) for i in range(1)]
                r_tln = [Res("tln%d" % i) for i in range(1)]
                blocks = [(1920, 512), (2432, 512), (2944, 128)]
                pbank = [0]

                def nb():
                    pbank[0] = (pbank[0] + 1) % 8
                    return pbank[0]

                r_crawh = [[Res("craw%d_%d" % (j, hf)) for hf in range(2)] for j in range(8)]
                wpre = {}
                for j in range(2):
                    wpre[j] = (load_w(j * 128), load_w(1024 + j * 128))
                for j in range(8):
                    wv, wg = wpre[j]
                    u = ub[j % 2]
                    ru = r_u[j % 2]
                    for bi, (t0, n) in enumerate(blocks):
                        bv, bg = nb(), nb()
                        for kc in range(16):
                            MM(bk(bv, 0, n), wbuf[wv][:, kc, :], xnT[:, kc, t0:t0 + n], kc == 0, kc == 15,
                               [r_wbuf[wv]] + r_xnT[t0 // 128:(t0 + n) // 128], [rP[bv]])
                        for kc in range(16):
                            MM(bk(bg, 0, n), wbuf[wg][:, kc, :], xnT[:, kc, t0:t0 + n], kc == 0, kc == 15,
                               [r_wbuf[wg]] + r_xnT[t0 // 128:(t0 + n) // 128], [rP[bg]])
                        emit_cv(1, [rP[bg].writer])
                        sg = sig[bi % 2]
                        ACT(sg[:, 0:n], bk(bg, 0, n), AF.Sigmoid, [rP[bg]], [r_sig[bi % 2]])
                        TT(u[:, t0 - 1920:t0 - 1920 + n], bk(bv, 0, n), sg[:, 0:n], ALU.mult,
                           [rP[bv], r_sig[bi % 2]], [ru])
                    if j + 2 < 8:
                        wpre[j + 2] = (load_w((j + 2) * 128), load_w(1024 + (j + 2) * 128))
                    for k in range(31):
                        for hf in range(2):
                            acc = craw[:, j, hf * 512:(hf + 1) * 512]
                            usl = u[:, 98 + k + hf * 512:98 + k + hf * 512 + 512]
                            rc = r_crawh[j][hf]
                            if k == 0:
                                TS(acc, usl, cw[:, j, 0:1], prm[:, j:j + 1], ALU.mult, ALU.add, [ru, r_cw, r_prm], [rc])
                            else:
                                STT(acc, usl, cw[:, j, k:k + 1], acc, ALU.mult, ALU.add, [ru, r_cw, rc], [rc])
                for blk in range(2):
                    c0 = blk * 512
                    bs, bq = nb(), nb()
                    for j in range(8):
                        MM(bk(bs), onesf[:], craw[:, j, c0:c0 + 512], j == 0, j == 7, [r_const] + r_crawh[j], [rP[bs]])
                    for j in range(8):
                        ACT(csq[:, 0:512], craw[:, j, c0:c0 + 512], AF.Square, r_crawh[j], [r_csq])
                        MM(bk(bq), onesf[:], csq[:, 0:512], j == 0, j == 7, [r_const, r_csq], [rP[bq]])
                    ACT(mean[:, c0:c0 + 512], bk(bs), AF.Copy, [rP[bs]], [r_mean], scale=1.0 / 1024)
                    TT(lrs[:, c0:c0 + 512], mean[:, c0:c0 + 512], mean[:, c0:c0 + 512], ALU.mult, [r_mean], [r_lrs])
                    STT(lrs[:, c0:c0 + 512], bk(bq), 1.0 / 1024, lrs[:, c0:c0 + 512], ALU.mult, ALU.subtract,
                        [rP[bq], r_lrs], [r_lrs])
                    ACT(lrs[:, c0:c0 + 512], lrs[:, c0:c0 + 512], AF.Ln, [r_lrs, r_const], [r_lrs], bias=epsb[:, 0:1])
                    ACT(lrs[:, c0:c0 + 512], lrs[:, c0:c0 + 512], AF.Exp, [r_lrs], [r_lrs], scale=-0.5)
                for j in range(8):
                    t = tln[0]
                    rt = r_tln[0]
                    TT(t[:], craw[:, j, :], mean[:], ALU.subtract, r_crawh[j] + [r_mean], [rt])
                    TT(t[:], t[:], lrs[:], ALU.mult, [rt, r_lrs], [rt])
                    ACT(convT[:, j, :], t[:], AF.Silu, [rt, r_prm], [r_convT[j]],
                        scale=prm[:, 8 + j:9 + j], bias=prm[:, 16 + j:17 + j])
                S.barrier()

            with Scope() as sat:
                qT = sb(sat, "qT", [128, TOK], BF16)
                kT = sb(sat, "kT", [128, NT], BF16)
                vT = sb(sat, "vT", [128, NT], BF16)
                r_qT = [Res("qT%d" % i) for i in range(2)]
                r_kT = [Res("kT%d" % i) for i in range(6)]
                r_vT = [Res("vT%d" % i) for i in range(6)]
                vt = sb(sat, "vt", [128, NKT, 2, 65], BF16)
                r_vt = [Res("vt%d" % i) for i in range(NKT)]
                vcol = sb(sat, "vcol", [128, NKT], F32)
                r_vcol = S.dres("vcol")
                bm = sb(sat, "bmt", [128, 1536], F32)
                r_bm = S.dres("bm")
                sqb = [sb(sat, "sqb%d" % i, [128, 512], BF16) for i in range(2)]
                r_sqb = [Res("sqb%d" % i) for i in range(2)]
                rs = [sb(sat, "rs%d" % i, [128, 512], F32) for i in range(2)]
                r_rs = [Res("rs%d" % i) for i in range(2)]
                tsc = [sb(sat, "tsc%d" % i, [128, 512], F32) for i in range(2)]
                r_tsc = [Res("tsc%d" % i) for i in range(2)]
                pt = [sb(sat, "pt%d" % i, [128, 512], BF16) for i in range(3)]
                r_pt = [Res("pt%d" % i) for i in range(3)]
                osb = sb(sat, "osb", [65, TOK], F32)
                r_osb = Res("osb")
                rden = sb(sat, "rden", [128, 512], F32)
                r_rden = Res("rden")
                selN = sb(sat, "selN", [65, 2, 128], F32)
                selD = sb(sat, "selD", [65, 2, 128], F32)
                r_sel = Res("sel")
                S.op("dve", lambda e: e.memset(selN[:], 0.0), (), [r_sel])
                S.op("dve", lambda e: e.memset(selD[:], 0.0), (), [r_sel])
                CP(selN[0:64, 0, 0:64], identf[0:64, 0:64], [r_const, r_sel], [r_sel])
                CP(selN[0:64, 1, 64:128], identf[0:64, 0:64], [r_const, r_sel], [r_sel])
                S.op("dve", lambda e: e.memset(selD[64:65, 0, 0:64], 1.0), [r_sel], [r_sel])
                S.op("dve", lambda e: e.memset(selD[64:65, 1, 64:128], 1.0), [r_sel], [r_sel])
                DMA("sync", vcol[:], vcol_d, (), [r_vcol], r_vcol)
                for h in range(2):
                    CP(vt[:, :, h, 64], vcol[:], [r_vcol], r_vt)
                scale = 1.0 / 8.0
                BP = (4, 5, 0, 1, 2, 3)
                BS = (2, 3)
                BN = 6
                BT = 7
                pcount = [0]
                scount = [0]
                pcnt = [0]
                cvc = [0]

                def qk_proj(wi, dst, rdst, t0, hp, gcol):
                    b = BP[pcount[0] % 6]
                    i2 = pcount[0] % 2
                    pcount[0] += 1
                    for kc in range(16):
                        MM(bk(b), wbuf[wi][:, kc, :], xnT[:, kc, t0:t0 + 512], kc == 0, kc == 15,
                           [r_wbuf[wi]] + r_xnT[t0 // 128:t0 // 128 + 4], [rP[b]])
                    cvc[0] += 1
                    if cvc[0] % 2 == 0:
                        emit_cv(1, [rP[b].writer])
                    ACT(sqb[i2][:], bk(b), AF.Square, [rP[b]], [r_sqb[i2]])

                    def part2():
                        MM(bk(BN), blockones[:], sqb[i2][:], True, True, [r_const, r_sqb[i2]], [rP[BN]])
                        ACT(rs[i2][:], bk(BN), AF.Ln, [rP[BN], r_const], [r_rs[i2]], scale=1.0 / 64,
                            bias=epsb[:, 0:1])
                        ACT(rs[i2][:], rs[i2][:], AF.Exp, [r_rs[i2]], [r_rs[i2]], scale=-0.5)
                        STT(dst, bk(b), prm[:, gcol:gcol + 1], rs[i2][:], ALU.mult, ALU.mult,
                            [rP[b], r_prm, r_rs[i2]], [rdst])
                    return part2

                for hp in range(8):
                    wq = load_w(2048 + hp * 128)
                    wk = load_w(3072 + hp * 128)
                    wv_ = load_w(4096 + hp * 128)
                    pend = None
                    for i in range(2):
                        p2 = qk_proj(wq, qT[:, i * 512:(i + 1) * 512], r_qT[i], 2048 + i * 512, hp, 24)
                        if pend is not None:
                            pend()
                        pend = p2
                    for i in range(6):
                        p2 = qk_proj(wk, kT[:, i * 512:(i + 1) * 512], r_kT[i], i * 512, hp, 25)
                        if pend is not None:
                            pend()
                        pend = p2
                    for i in range(6):
                        b = BP[pcount[0] % 6]
                        pcount[0] += 1
                        t0 = i * 512
                        for kc in range(16):
                            MM(bk(b), wbuf[wv_][:, kc, :], xnT[:, kc, t0:t0 + 512], kc == 0, kc == 15,
                               [r_wbuf[wv_]] + r_xnT[t0 // 128:t0 // 128 + 4], [rP[b]])
                        cvc[0] += 1
                        if cvc[0] % 2 == 0:
                            emit_cv(1, [rP[b].writer])
                        CP(vT[:, t0:t0 + 512], bk(b), [rP[b]], [r_vT[i]], eng="act")
                        if pend is not None:
                            pend()
                            pend = None
                    for g0 in range(0, NKT, 8):
                        n = min(8, NKT - g0)
                        for ti in range(n):
                            s0, stp = KT[g0 + ti]
                            TR(psb[:, BT * 1024 + ti * 128: BT * 1024 + (ti + 1) * 128],
                               vT[:, ssl(s0, 128, stp)], identb[:], r_vT + [r_const], [rP[BT]])
                        CP(vt[:, g0:g0 + n, :, 0:64],
                           psb[:, BT * 1024: BT * 1024 + n * 128].rearrange("p (t h d) -> p t h d", t=n, h=2),
                           [rP[BT]], r_vt[g0:g0 + n], eng="dve")
                    for h in range(2):
                        hs = slice(h * 64, (h + 1) * 64)
                        DMA("sync", bm[:], bm_d[2 * hp + h], (), [r_bm], r_bm)
                        first = [True, True]
                        pv_pend = [None]

                        def score_group(items, bmcol, h=h, hs=hs):
                            b = BS[scount[0] % 2]
                            i2 = scount[0] % 2
                            ip = pcnt[0] % 3
                            scount[0] += 1
                            pcnt[0] += 1
                            for (kt, (q0, qn, qs), off) in items:
                                s0, stp = KT[kt]
                                MM(bk(b, off, qn), kT[hs, ssl(s0, 128, stp)], qT[hs, ssl(q0, qn, qs)],
                                   True, True, r_kT + r_qT, [rP[b]])
                            STT(tsc[i2][:], bk(b), scale, bm[:, bmcol:bmcol + 512], ALU.mult, ALU.add,
                                [rP[b], r_bm], [r_tsc[i2]])
                            ACT(pt[ip][:], tsc[i2][:], AF.Exp, [r_tsc[i2]], [r_pt[ip]])
                            prev = pv_pend[0]
                            pv_pend[0] = lambda: pv_part(items, ip, h)
                            if prev is not None:
                                prev()

                        def pv_part(items, ip, h):
                            for (kt, (q0, qn, qs), off) in items:
                                segs = []
                                if q0 + (qn - 1) * qs < 512 or q0 >= 512:
                                    segs.append((0, qn))
                                else:
                                    n0 = (512 - q0 + qs - 1) // qs
                                    segs.append((0, n0))
                                    segs.append((n0, qn - n0))
                                for (i0, nn) in segs:
                                    c0 = q0 + i0 * qs
                                    ob = c0 // 512
                                    cc = c0 % 512
                                    MM(ps[0:65, ob * 512 + cc: ob * 512 + cc + (nn - 1) * qs + 1: qs],
                                       vt[:, kt, h, :], pt[ip][:, off + i0: off + i0 + nn],
                                       first[ob], False, [r_vt[kt], r_pt[ip]], [rP[ob]])
                                    first[ob] = False

                        for b2 in range(0, 8, 2):
                            items = []
                            for x in range(2):
                                qb = b2 + x
                                items.append((qb, (qb * 128, 128, 1), x * 256))
                                items.append((qb + 1, (qb * 128, 128, 1), x * 256 + 128))
                            score_group(items, 0)
                        for r in range(4):
                            items = []
                            for x in range(2):
                                items.append((9 + 3 * r + x, (512 * x + r, 128, 4), x * 256))
                                items.append((9 + 3 * r + x + 1, (512 * x + r, 128, 4), x * 256 + 128))
                            score_group(items, 512)
                        for r4 in range(0, 16, 4):
                            items = []
                            for x in range(4):
                                r = r4 + x
                                items.append((21 + 2 * r, (r, 64, 16), x * 128))
                                items.append((21 + 2 * r + 1, (r, 64, 16), x * 128 + 64))
                            score_group(items, 1024)
                        pv_pend[0]()
                        pv_pend[0] = None
                        CP(osb[:, :], ps[0:65, 0:1024], [rP[0], rP[1]], [r_osb], eng="act")
                        for blk in range(2):
                            c0 = blk * 512
                            bN = (4, 5)[pcount[0] % 2]
                            pcount[0] += 1
                            MM(bk(BT), selD[:, h, :], osb[:, c0:c0 + 512], True, True, [r_sel, r_osb], [rP[BT]])
                            MM(bk(bN), selN[:, h, :], osb[:, c0:c0 + 512], True, True, [r_sel, r_osb], [rP[bN]])
                            S.op("dve", lambda e, c0=c0, hs=hs: e.reciprocal(out=rden[hs, :], in_=bk(BT)[hs, :]),
                                 [rP[BT]], [r_rden])
                            TT(attnT[hs, hp, c0:c0 + 512], bk(bN)[hs, :], rden[hs, :], ALU.mult,
                               [rP[bN], r_rden], [r_attnT[hp]])
                S.barrier()

        with Scope() as s2:
            hn = sb(s2, "hn", [128, 8, D], BF16)
            r_hn = [Res("hn%d" % i) for i in range(8)]
            oh1 = sb(s2, "oh1", [128, 8, 32], F32)
            oh2 = sb(s2, "oh2", [128, 8, 32], F32)
            m32 = sb(s2, "m32", [128, 8, 32], BF16)
            g32 = sb(s2, "g32", [128, 8, 32], F32)
            rank = sb(s2, "rank", [128, 8, 32], F32)
            rankm = sb(s2, "rankm", [128, 8, 32], F32)
            m32f = sb(s2, "m32f", [128, 8, 32], F32)
            r_rt = [Res("route%d" % i) for i in range(8)]
            r_rank = Res("rank")
            idx = sb(s2, "idx", [128, 8, 2], I32)
            gsel = sb(s2, "gsel", [128, 8, 2], F32)
            r_idx = Res("idx")

            with Scope() as so:
                wo = sb(so, "wo", [128, 16, D], BF16)
                r_wo = [S.dres("wo%d" % i) for i in range(4)]
                w_out_v = w_out.rearrange("(kc p) n -> p kc n", p=128)
                for i in range(4):
                    DMA("pool", wo[:, :, i * 512:(i + 1) * 512], w_out_v[:, :, i * 512:(i + 1) * 512], (),
                        [r_wo[i]], r_wo[i])
                emit_cv(len(cv_list))
                g2bc = sb(so, "g2bc", [128, D], F32)
                r_g2 = S.dres("g2bc")
                DMA("sync", g2bc[:], g2.broadcast_to([128, D]), (), [r_g2], r_g2)
                wr = sb(so, "wr", [128, 16, 36], F32)
                r_wr = S.dres("wr")
                DMA("sync", wr[:], wr_d.rearrange("(kc p) n -> p kc n", p=128), (), [r_wr], r_wr)
                for kc in range(16):
                    TS(wr[:, kc, :], wr[:, kc, :], prm[:, 26 + kc:27 + kc], None, ALU.mult, ALU.bypass,
                       [r_wr, r_prm], [r_wr])
                brbc = sb(so, "brbc", [128, 36], F32)
                r_br = S.dres("brbc")
                DMA("sync", brbc[:], br_d.broadcast_to([128, 36]), (), [r_br], r_br)
                xr = [sb(so, "xr%d" % i, [128, D], F32) for i in range(2)]
                r_xr = [S.dres("xr%d" % i) for i in range(2)]
                x1 = [sb(so, "x1_%d" % i, [128, D], F32) for i in range(2)]
                r_x1 = [S.dres("x1_%d" % i) for i in range(2)]
                junk2 = sb(so, "junk2", [128, D], BF16)
                r_junk2 = Res("junk2")
                x1T = sb(so, "x1T", [128, 16, 128], F32)
                r_x1T = Res("x1T")
                st2 = sb(so, "st2", [128, 8, 4], F32)
                lg = sb(so, "lg", [128, 36], F32)
                r_lg = Res("lg")
                sc8 = sb(so, "sc8", [128, 64], F32)
                r_sc = Res("sc8")
                mixT = lambda c: (convT[:, c, :] if c < 8 else attnT[:, c - 8, :])
                r_mix = r_convT + r_attnT
                r_out_rows = [Res("outrows%d" % i) for i in range(8)]
                r_st2 = [Res("st2_%d" % i) for i in range(8)]

                def stage_a(tt):
                    b = tt % 2
                    DMA("sync", xr[b][:], xh[HALO + tt * 128: HALO + (tt + 1) * 128, :], (), [r_xr[b]], r_xr[b])
                    for cb in range(4):
                        for c in range(16):
                            MM(bk(cb), mixT(c)[:, tt * 128:(tt + 1) * 128], wo[:, c, cb * 512:(cb + 1) * 512],
                               c == 0, c == 15, [r_mix[c], r_wo[cb]], [rP[cb]])
                    TT(x1[b][:], ps[:, 0:2048], xr[b][:], ALU.add, rP[0:4] + [r_xr[b]], [r_x1[b]])
                    DMA("sync", out_d[tt * 128:(tt + 1) * 128, :], x1[b][:], [r_x1[b]], [r_out_rows[tt]], r_x1[b])
                    ACT(junk2[:], x1[b][:], AF.Square, [r_x1[b]], [r_junk2, r_st2[tt]], accum_out=st2[:, tt, 0:1])
                    ACT(st2[:, tt, 1:2], st2[:, tt, 0:1], AF.Ln, [r_st2[tt], r_const], [r_st2[tt]], scale=1.0 / D,
                        bias=epsb[:, 0:1])
                    ACT(st2[:, tt, 1:2], st2[:, tt, 1:2], AF.Exp, [r_st2[tt]], [r_st2[tt]], scale=-0.5)
                    STT(hn[:, tt, :], x1[b][:], st2[:, tt, 1:2], g2bc[:], ALU.mult, ALU.mult,
                        [r_x1[b], r_st2[tt], r_g2], [r_hn[tt]])

                def stage_b(tt):
                    b = tt % 2
                    for half in range(2):
                        for j in range(8):
                            kc = half * 8 + j
                            TR(ps[:, (4 + half * 2) * 512 + j * 128:(4 + half * 2) * 512 + (j + 1) * 128],
                               x1[b][:, kc * 128:(kc + 1) * 128], identf[:], [r_x1[b], r_const],
                               [rP[4 + half * 2], rP[5 + half * 2]])
                        CP(x1T[:, half * 8:(half + 1) * 8, :],
                           ps[:, (4 + half * 2) * 512:(6 + half * 2) * 512].rearrange("p (j t) -> p j t", j=8),
                           [rP[4 + half * 2], rP[5 + half * 2]], [r_x1T], eng=("act" if half == 0 else "dve"))
                    for kc in range(16):
                        MM(bk(4, 0, 36), x1T[:, kc, :], wr[:, kc, :], kc == 0, kc == 15, [r_x1T, r_wr], [rP[4]])
                    STT(lg[:], bk(4, 0, 36), st2[:, tt, 1:2], brbc[:], ALU.mult, ALU.add, [rP[4], r_st2[tt], r_br], [r_lg])
                    R = [r_lg, r_sc, r_rt[tt]]
                    W = [r_sc, r_rt[tt]]
                    gmax = sc8[:, 0:1]
                    S.op("dve", lambda e: e.tensor_reduce(out=sc8[:, 0:1], in_=lg[:, 0:4], axis=AX.X, op=ALU.max), R, W)
                    TS(sc8[:, 4:8], lg[:, 0:4], sc8[:, 0:1], None, ALU.is_equal, ALU.bypass, R, W)
                    TS(sc8[:, 1:2], sc8[:, 0:1], -1.0, None, ALU.mult, ALU.bypass, R, W)
                    ACT(sc8[:, 8:12], lg[:, 0:4], AF.Exp, R, W, bias=sc8[:, 1:2])
                    S.op("dve", lambda e: e.tensor_reduce(out=sc8[:, 2:3], in_=sc8[:, 8:12], axis=AX.X, op=ALU.add), R, W)
                    S.op("dve", lambda e: e.reciprocal(out=sc8[:, 3:4], in_=sc8[:, 2:3]), R, W)
                    TS(sc8[:, 16:24], lg[:, 4:12], sc8[:, 4:5], None, ALU.mult, ALU.bypass, R, W)
                    for g in range(1, 4):
                        STT(sc8[:, 16:24], lg[:, 4 + 8 * g:12 + 8 * g], sc8[:, 4 + g:5 + g], sc8[:, 16:24],
                            ALU.mult, ALU.add, R, W)
                    S.op("dve", lambda e: e.max(out=sc8[:, 24:32], in_=sc8[:, 16:24]), R, W)
                    TS(sc8[:, 32:40], sc8[:, 16:24], sc8[:, 24:25], None, ALU.is_equal, ALU.bypass, R, W)
                    TS(sc8[:, 40:48], sc8[:, 16:24], sc8[:, 25:26], None, ALU.is_equal, ALU.bypass, R, W)
                    TT(sc8[:, 12:13], sc8[:, 24:25], sc8[:, 25:26], ALU.subtract, R, W)
                    ACT(sc8[:, 12:13], sc8[:, 12:13], AF.Exp, R, W)
                    TS(sc8[:, 12:13], sc8[:, 12:13], 1.0, None, ALU.add, ALU.bypass, R, W)
                    S.op("dve", lambda e: e.reciprocal(out=sc8[:, 13:14], in_=sc8[:, 12:13]), R, W)
                    TS(sc8[:, 14:15], sc8[:, 13:14], -1.0, 1.0, ALU.mult, ALU.add, R, W)
                    TT(sc8[:, 13:15], sc8[:, 13:15], sc8[:, 3:4].to_broadcast([128, 2]), ALU.mult, R, W)
                    for g in range(4):
                        TS(oh1[:, tt, 8 * g:8 * g + 8], sc8[:, 32:40], sc8[:, 4 + g:5 + g], None, ALU.mult, ALU.bypass, R, W)
                        TS(oh2[:, tt, 8 * g:8 * g + 8], sc8[:, 40:48], sc8[:, 4 + g:5 + g], None, ALU.mult, ALU.bypass, R, W)
                    TT(m32[:, tt, :], oh1[:, tt, :], oh2[:, tt, :], ALU.add, R, W)
                    TT(m32f[:, tt, :], oh1[:, tt, :], oh2[:, tt, :], ALU.add, R, W)
                    TS(g32[:, tt, :], oh1[:, tt, :], sc8[:, 14:15], None, ALU.mult, ALU.bypass, R, W)
                    STT(g32[:, tt, :], oh2[:, tt, :], sc8[:, 13:14], g32[:, tt, :], ALU.mult, ALU.add, R, W)

                stage_a(0)
                for tt in range(8):
                    if tt + 1 < 8:
                        stage_a(tt + 1)
                    stage_b(tt)
                for tt in range(8):
                    for j in range(tt + 1):
                        MM(bk(5, tt * 32, 32), (ustrict[:] if j == tt else onesb[:]), m32[:, j, :], j == 0, j == tt,
                           [r_const, r_rt[j]], [rP[5]])
                CP(rank[:], bk(5, 0, 256).rearrange("p (t e) -> p t e", t=8), [rP[5]], [r_rank])
                STT(rankm[:], rank[:], 1.0, m32f[:], ALU.add, ALU.mult, r_rt + [r_rank], [r_rank])
                TS(rankm[:], rankm[:], -1.0, None, ALU.add, ALU.bypass, [r_rank], [r_rank])
                sl = sb(so, "sl", [128, 8, 32], F32)
                ok = sb(so, "ok", [128, 8, 32], F32)
                red = sb(so, "red", [128, 8, 4], F32)
                r_sl = Res("sl")
                Rr = r_rt + [r_rank, r_sl, r_const]
                Wr_ = [r_sl]
                TT(sl[:], rank[:], ebase[:].unsqueeze(1).to_broadcast([128, 8, 32]), ALU.add, Rr, Wr_)
                TS(ok[:], rank[:], float(CAP), None, ALU.is_lt, ALU.bypass, Rr, Wr_)
                STT(sl[:], sl[:], -float(ZROW), ok[:], ALU.add, ALU.mult, Rr, Wr_)
                TS(sl[:], sl[:], float(ZROW), None, ALU.add, ALU.bypass, Rr, Wr_)
                for k, oh in enumerate((oh1, oh2)):
                    TT(ok[:], sl[:], oh[:], ALU.mult, Rr, Wr_)
                    S.op("dve", lambda e, k=k: e.tensor_reduce(out=red[:, :, k], in_=ok[:], axis=AX.X, op=ALU.add), Rr, Wr_)
                    TT(ok[:], g32[:], oh[:], ALU.mult, Rr, Wr_)
                    S.op("dve", lambda e, k=k: e.tensor_reduce(out=gsel[:, :, k], in_=ok[:], axis=AX.X, op=ALU.add),
                         Rr, Wr_ + [r_idx])
                CP(idx[:], red[:, :, 0:2], Rr, [r_idx])
                S.barrier()

            if STOP_AFTER == "mixer":
                S.finish([(r.dsem, r.dcount, "dma") for r in S.dsems if r.dcount > 0])
                S.replay()
                return nc

            with Scope() as sm:
                NSTG = 3
                wgb = [sb(sm, "wgb%d" % i, [128, 16, 256], BF16) for i in range(NSTG)]
                wub = [sb(sm, "wub%d" % i, [128, 16, 256], BF16) for i in range(NSTG)]
                wdb = [sb(sm, "wdb%d" % i, [128, 2, D], BF16) for i in range(NSTG)]
                r_wgb = [S.dres("wgb%d" % i) for i in range(NSTG)]
                r_wub = [S.dres("wub%d" % i) for i in range(NSTG)]
                r_wdb = [S.dres("wdb%d" % i) for i in range(NSTG)]
                se = [sb(sm, "se%d" % i, [128, 8, 128], BF16) for i in range(2)]
                r_se = [Res("se%d" % i) for i in range(2)]
                xe = [sb(sm, "xe%d" % i, [128, 16, 128], BF16) for i in range(2)]
                r_xe = [Res("xe%d" % i) for i in range(2)]
                sgt = [sb(sm, "sgt%d" % i, [128, 256], F32) for i in range(2)]
                r_sgt = [Res("sgt%d" % i) for i in range(2)]
                hT = [sb(sm, "hT%d" % i, [128, 2, 128], BF16) for i in range(2)]
                r_hT = [Res("hT%d" % i) for i in range(2)]
                yb = [sb(sm, "yb%d" % i, [128, D], F32) for i in range(2)]
                r_yb = [S.dres("yb%d" % i) for i in range(2)]
                r_yd = Res("yd")
                y1b = sb(sm, "y1b", [128, D], F32)
                y2b = sb(sm, "y2b", [128, D], F32)
                zt = y1b
                r_zt = S.dres("zt")
                S.op("dve", lambda e: e.memset(zt[:], 0.0), (), [r_zt])
                DMA("sync", yd[ZROW:ZROW + 128, :], zt[:], [r_zt], [r_yd], r_zt)
                stage = 0
                order = []
                ia, ib = 0, NCV
                while ia < NCV or ib < NE:
                    if ia < NCV:
                        order.append(ia)
                        ia += 1
                    if ib < NE:
                        order.append(ib)
                        ib += 1
                    if ib < NE and len(order) % 3 == 2:
                        order.append(ib)
                        ib += 1
                assert sorted(order) == list(range(NE))
                for ei, ex in enumerate(order):
                    e2 = ei % 2
                    for tt in range(8):
                        TS(se[e2][:, tt, :], iota_row[:], rankm[:, tt, ex:ex + 1], None,
                           ALU.is_equal, ALU.bypass, [r_const, r_rank, r_rt[tt]], [r_se[e2]])
                    for g4 in range(4):
                        gb = 6 + (g4 % 2)
                        for c in range(4):
                            kc = g4 * 4 + c
                            for tt in range(8):
                                MM(bk(gb, c * 128, 128), hn[:, tt, kc * 128:(kc + 1) * 128], se[e2][:, tt, :],
                                   tt == 0, tt == 7, [r_hn[tt], r_se[e2]], [rP[gb]])
                        CP(xe[e2][:, g4 * 4:(g4 + 1) * 4, :], bk(gb).rearrange("p (c s) -> p c s", c=4),
                           [rP[gb]], [r_xe[e2]], eng=("act" if g4 % 2 == 0 else "dve"))
                    for fq in range(4):
                        sgi = stage % NSTG
                        sg_, su_, sd_ = wgc, wu_d, (wdc if ex < NCV else wd_d)
                        rcv = []
                        DMA("pool", wgb[sgi][:], sg_[ex].rearrange("(kc p) f -> p kc f", p=128)[:, :, fq * 256:(fq + 1) * 256],
                            rcv, [r_wgb[sgi]], r_wgb[sgi])
                        DMA("pool", wub[sgi][:], su_[ex].rearrange("(kc p) f -> p kc f", p=128)[:, :, fq * 256:(fq + 1) * 256],
                            rcv, [r_wub[sgi]], r_wub[sgi])
                        DMA("pool", wdb[sgi][:], sd_[ex, fq * 256:(fq + 1) * 256, :].rearrange("(fc p) d -> p fc d", p=128),
                            rcv, [r_wdb[sgi]], r_wdb[sgi])
                        hb = 4 + (stage % 2)
                        h2 = stage % 2
                        for fc in range(2):
                            for kc in range(16):
                                MM(bk(hb, fc * 128, 128), wgb[sgi][:, kc, fc * 128:(fc + 1) * 128], xe[e2][:, kc, :],
                                   kc == 0, kc == 15, [r_wgb[sgi], r_xe[e2]], [rP[hb]])
                            for kc in range(16):
                                MM(bk(hb, 256 + fc * 128, 128), wub[sgi][:, kc, fc * 128:(fc + 1) * 128], xe[e2][:, kc, :],
                                   kc == 0, kc == 15, [r_wub[sgi], r_xe[e2]], [rP[hb]])
                        ACT(sgt[h2][:], bk(hb, 0, 256), AF.Silu, [rP[hb]], [r_sgt[h2]])
                        TT(hT[h2][:].rearrange("p c s -> p (c s)"), sgt[h2][:], bk(hb, 256, 256), ALU.mult,
                           [r_sgt[h2], rP[hb]], [r_hT[h2]])
                        for fc in range(2):
                            for cb in range(4):
                                MM(bk(cb), hT[h2][:, fc, :], wdb[sgi][:, fc, cb * 512:(cb + 1) * 512],
                                   fq == 0 and fc == 0, fq == 3 and fc == 1, [r_hT[h2], r_wdb[sgi]], [rP[cb]])
                        stage += 1
                    CP(yb[e2][:], ps[:, 0:2048], rP[0:4], [r_yb[e2]], eng="act")
                    DMA("sync", yd[ex * CAP:(ex + 1) * CAP, :], yb[e2][:], [r_yb[e2]], [r_yd], r_yb[e2])
                with Scope() as sf:
                    y1 = [sb(sf, "y1a", [128, D], F32, at=attn_off + 16384), y1b]
                    y2 = [sb(sf, "y2a", [128, D], F32, at=attn_off + 24576), y2b]
                    xo = [sb(sf, "xo0", [128, D], F32, at=attn_off), sb(sf, "xo1", [128, D], F32, at=attn_off + 8192)]
                    r_y1 = [S.dres("y1_0"), r_zt]
                    r_y2 = [S.dres("y2_%d" % i) for i in range(2)]
                    r_xo = [S.dres("xo%d" % i) for i in range(2)]
                    fin = []
                    for tt in range(8):
                        b = tt % 2
                        DMA("sync", xo[b][:], out_d[tt * 128:(tt + 1) * 128, :], [r_out_rows[tt]], [r_xo[b]], r_xo[b])
                        S.op("pool", lambda e, tt=tt, b=b: e.indirect_dma_start(
                            out=y1[b][:], out_offset=None, in_=yd,
                            in_offset=bass.IndirectOffsetOnAxis(ap=idx[:, tt, 0:1], axis=0),
                            bounds_check=NE * CAP + 127, oob_is_err=False), [r_idx, r_yd], [r_y1[b]], dma=r_y1[b])
                        S.op("pool", lambda e, tt=tt, b=b: e.indirect_dma_start(
                            out=y2[b][:], out_offset=None, in_=yd,
                            in_offset=bass.IndirectOffsetOnAxis(ap=idx[:, tt, 1:2], axis=0),
                            bounds_check=NE * CAP + 127, oob_is_err=False), [r_idx, r_yd], [r_y2[b]], dma=r_y2[b])
                        STT(xo[b][:], y1[b][:], gsel[:, tt, 0:1], xo[b][:], ALU.mult, ALU.add,
                            [r_y1[b], r_idx, r_xo[b]], [r_xo[b]])
                        STT(xo[b][:], y2[b][:], gsel[:, tt, 1:2], xo[b][:], ALU.mult, ALU.add,
                            [r_y2[b], r_idx, r_xo[b]], [r_xo[b]])
                        fin.append(DMA("sync", out_d[tt * 128:(tt + 1) * 128, :], xo[b][:], [r_xo[b]],
                                       [r_out_rows[tt]], r_xo[b]))
                    S.finish(fin)
                    S.replay()
    return nc


_CACHE = {}


def prep_inputs(x, norm1_g, w_in, q_norm_g, k_norm_g, conv_w, conv_b, conv_ln_g, conv_ln_b,
                rel_bias, w_out, norm2_g, w_router_group, b_router_group, w_router_expert,
                b_router_expert, w_gate, w_up, w_down):
    f = lambda a: np.ascontiguousarray(np.asarray(a, dtype=np.float32))
    x = f(x)[0]
    xpad = np.concatenate([np.zeros((HALO, D), np.float32), x], axis=0)
    pst = np.zeros((48, 128), np.float32)
    pst[0:8] = f(conv_b)[0].reshape(8, 128)
    pst[8:16] = f(conv_ln_g)[0].reshape(8, 128)
    pst[16:24] = f(conv_ln_b)[0].reshape(8, 128)
    pst[24] = np.tile(f(q_norm_g)[0], 2)
    pst[25] = np.tile(f(k_norm_g)[0], 2)
    pst[26:42] = f(norm2_g)[0].reshape(16, 128)
    bidx, mask = _bucket_tables()
    rb = f(rel_bias)
    bm = np.where(mask[None], rb[bidx].transpose(2, 0, 1), np.float32(NEG)).astype(np.float32)
    wr = np.concatenate([f(w_router_group)[0]] + [f(w_router_expert)[0, g] for g in range(4)], axis=1)
    br = np.concatenate([f(b_router_group)[0], f(b_router_expert)[0].reshape(-1)])[None, :]
    shared = {
        "w_in": f(w_in)[0], "w_out": f(w_out)[0], "pst": pst, "g1": f(norm1_g), "g2": f(norm2_g),
        "convw": f(conv_w)[0], "bm": bm, "wr": np.ascontiguousarray(wr), "br": np.ascontiguousarray(br),
        "wg": f(w_gate)[0].reshape(NE, D, 1024), "wu": f(w_up)[0].reshape(NE, D, 1024),
        "wd": f(w_down)[0].reshape(NE, 1024, D),
    }
    in_maps = []
    for c in range(NCORES):
        m = dict(shared)
        m["xh"] = np.ascontiguousarray(xpad[TOK * c: TOK * c + NT])
        first_valid = HALO - TOK * c
        vc = np.zeros((128, NKT), np.float32)
        for ti, (s0, stp) in enumerate(KT):
            tp = s0 + stp * np.arange(128)
            vc[:, ti] = (tp >= first_valid).astype(np.float32)
        m["vcol"] = vc
        in_maps.append(m)
    return in_maps


def kernel(**inputs):
    in_maps = prep_inputs(**inputs)
    if "nc" not in _CACHE:
        _CACHE["nc"] = build_program()
    nc = _CACHE["nc"]
    res = run_bass_kernel_spmd(nc, in_maps, core_ids=list(range(NCORES)))
    out = np.concatenate([r["out"] for r in res.results], axis=0)
    return out.reshape(1, SEQ, D).astype(np.float32)
```

```python
import math
from contextlib import ExitStack
import numpy as np
import concourse.bass as bass
import concourse.mybir as mybir
from concourse.bass_utils import run_bass_kernel_spmd

F32 = mybir.dt.float32
BF16 = mybir.dt.bfloat16
I32 = mybir.dt.int32
ALU = mybir.AluOpType
AF = mybir.ActivationFunctionType
AX = mybir.AxisListType

ENGS = ("sync", "act", "pool", "pe", "dve")
NCORES = 8
SEQ = 8192
D = 2048
TOK = SEQ // NCORES
HALO = 2048
NT = HALO + TOK
NE = 32
CAP = 128
EPS = 1e-6
NEG = -30000.0
ZROW = NE * CAP
STOP_AFTER = None
NCV = 12


class Res:
    __slots__ = ("name", "writer", "readers", "dsem", "dcount")

    def __init__(self, name, dsem=None):
        self.name = name
        self.writer = None
        self.readers = {}
        self.dsem = dsem
        self.dcount = 0


class Sched:
    def __init__(self, nc, stack):
        self.nc = nc
        self.stack = stack
        self.q = {e: [] for e in ENGS}
        self.cnt = {e: 0 for e in ENGS}
        self.seen = {e: {} for e in ENGS}
        self.sem = {e: stack.enter_context(nc.semaphore("s_" + e)) for e in ENGS}
        self.final_toks = []
        self.dsems = []

    def dres(self, name):
        s = self.stack.enter_context(self.nc.semaphore("d_" + name))
        r = Res(name, dsem=s)
        self.dsems.append(r)
        return r

    def op(self, eng, fn, reads=(), writes=(), dma=None):
        need = {}

        def add(tok, war=False):
            if tok is None:
                return
            sem, val, teng = tok
            if teng == eng and teng == "pe":
                return
            k = id(sem)
            if k not in need or need[k][1] < val:
                need[k] = (sem, val)

        for r in reads:
            add(r.writer)
        for w in writes:
            add(w.writer)
            for tok in w.readers.values():
                add(tok, war=True)
        waits = []
        seen = self.seen[eng]
        for k, (sem, val) in need.items():
            if seen.get(k, 0) >= val:
                continue
            seen[k] = val
            waits.append((sem, val))
        if dma is None:
            self.cnt[eng] += 1
            tok = (self.sem[eng], self.cnt[eng], eng)
            inc = (self.sem[eng], 1)
        else:
            dma.dcount += 16
            tok = (dma.dsem, dma.dcount, "dma")
            inc = (dma.dsem, 16)
        self.q[eng].append((waits, fn, inc))
        for r in reads:
            k = id(tok[0])
            old = r.readers.get(k)
            if old is None or old[1] < tok[1]:
                r.readers[k] = tok
        for w in writes:
            w.writer = tok
            w.readers = {}
        return tok

    def barrier(self):
        toks = [(self.sem[e], self.cnt[e], e) for e in ENGS if self.cnt[e] > 0]
        toks += [(r.dsem, r.dcount, "dma") for r in self.dsems if r.dcount > 0]
        for eng in ENGS:
            waits = []
            seen = self.seen[eng]
            for sem, val, teng in toks:
                if teng == eng:
                    continue
                k = id(sem)
                if seen.get(k, 0) >= val:
                    continue
                seen[k] = val
                waits.append((sem, val))
            if waits:
                self.q[eng].append((waits, None, None))

    def finish(self, toks):
        self.final_toks = list(toks)

    def replay(self):
        nc = self.nc
        q = self.q
        final = self.final_toks

        def run(engobj, items):
            for waits, fn, inc in items:
                for sem, val in waits:
                    engobj.wait_ge(sem, val)
                if fn is None:
                    continue
                ins = fn(engobj)
                ins.then_inc(inc[0], inc[1])

        with nc.Block() as block:
            @block.sync
            def _(e):
                run(e, q["sync"])
                for sem, val, _t in final:
                    e.wait_ge(sem, val)

            @block.scalar
            def _(e):
                run(e, q["act"])

            @block.gpsimd
            def _(e):
                run(e, q["pool"])

            @block.tensor
            def _(e):
                run(e, q["pe"])

            @block.vector
            def _(e):
                run(e, q["dve"])


def _t5_bucket_np(dist):
    max_exact = 16
    nf = np.maximum(dist, 1).astype(np.float32)
    large = max_exact + (np.log(nf / np.float32(max_exact)) / np.float32(math.log(2048 / max_exact))
                         * np.float32(32 - max_exact)).astype(np.int32)
    large = np.minimum(large, 31)
    return np.where(dist < max_exact, dist, large)


def _bucket_tables():
    kk = np.arange(128)[:, None]
    bidx = np.zeros((128, 1536), np.int64)
    mask = np.zeros((128, 1536), bool)
    for pi, d in enumerate((1, 4)):
        q = np.arange(128)[None, :]
        relA = 128 + q - kk
        relB = q - kk
        for rep in range(2):
            c0 = pi * 512 + rep * 256
            bidx[:, c0:c0 + 128] = _t5_bucket_np(np.clip(relA, 0, 128) * d)
            mask[:, c0:c0 + 128] = (relA >= 0) & (relA <= 128)
            bidx[:, c0 + 128:c0 + 256] = _t5_bucket_np(np.clip(relB, 0, 128) * d)
            mask[:, c0 + 128:c0 + 256] = (relB >= 0) & (relB <= 128)
    q = np.arange(64)[None, :]
    relA = 128 + q - kk
    relB = q + 64 - kk
    for rep in range(4):
        c0 = 1024 + rep * 128
        bidx[:, c0:c0 + 64] = _t5_bucket_np(np.clip(relA, 0, 128) * 16)
        mask[:, c0:c0 + 64] = (relA >= 0) & (relA <= 128)
        bidx[:, c0 + 64:c0 + 128] = _t5_bucket_np(np.clip(relB, 0, 128) * 16)
        mask[:, c0 + 64:c0 + 128] = (relB >= 0) & (relB <= 128) & (kk >= 64)
    return bidx, mask


def _key_tiles():
    tiles = []
    for i in range(9):
        tiles.append((1920 + 128 * i, 1))
    for r in range(4):
        for i in range(3):
            tiles.append((2048 - 512 + r + 512 * i, 4))
    for r in range(16):
        tiles.append((r, 16))
        tiles.append((1024 + r, 16))
    return tiles


KT = _key_tiles()
NKT = len(KT)


def build_program():
    nc = bass.Bass("TRN2", target_bir_lowering=False)
    dram = lambda name, shape, dt, kind="ExternalInput": nc.dram_tensor(name, list(shape), dt, kind=kind).ap()
    xh = dram("xh", [NT, D], F32)
    w_in = dram("w_in", [D, 5120], F32)
    w_out = dram("w_out", [D, D], F32)
    pst = dram("pst", [48, 128], F32)
    g1 = dram("g1", [1, D], F32)
    g2 = dram("g2", [1, D], F32)
    convw = dram("convw", [31, 1024], F32)
    bm_d = dram("bm", [16, 128, 1536], F32)
    vcol_d = dram("vcol", [128, NKT], F32)
    wr_d = dram("wr", [D, 36], F32)
    br_d = dram("br", [1, 36], F32)
    wg_d = dram("wg", [NE, D, 1024], F32)
    wu_d = dram("wu", [NE, D, 1024], F32)
    wd_d = dram("wd", [NE, 1024, D], F32)
    out_d = dram("out", [TOK, D], F32, kind="ExternalOutput")
    yd = dram("yd", [NE * CAP + 128, D], F32, kind="Internal")
    wgc = dram("wgc", [NE, D, 1024], BF16, kind="Internal")
    wdc = dram("wdc", [NCV, 1024, D], BF16, kind="Internal")

    with ExitStack() as st:
        S = Sched(nc, st)

        ARENA_BYTES = 212736
        arena = st.enter_context(nc.sbuf_tensor("arena", [128, ARENA_BYTES // 2], BF16))
        ar = {"lo": 0, "hi": ARENA_BYTES}

        def _view(off, shape, dt):
            esz = 2 if dt == BF16 else 4
            nel = 1
            for d_ in shape[1:]:
                nel *= d_
            v = arena[0:shape[0], off // 2:(off + nel * esz) // 2]
            if dt != BF16:
                v = v.bitcast(dt)
            if len(shape) == 3:
                v = v.rearrange("p (a b) -> p a b", a=shape[1])
            elif len(shape) == 4:
                v = v.rearrange("p (a b c) -> p a b c", a=shape[1], b=shape[2])
            return v

        def _nbytes(shape, dt):
            nel = 1
            for d_ in shape[1:]:
                nel *= d_
            return ((nel * (2 if dt == BF16 else 4)) + 63) // 64 * 64

        class Scope:
            def __enter__(self):
                self.mark = ar["lo"]
                return self

            def __exit__(self, *a):
                ar["lo"] = self.mark
                return False

        def sb(stack, name, shape, dt, at=None):
            nb = _nbytes(shape, dt)
            if at is not None:
                return _view(at, shape, dt)
            if stack is st:
                ar["hi"] -= nb
                off = ar["hi"]
            else:
                off = ar["lo"]
                ar["lo"] += nb
            assert ar["lo"] <= ar["hi"], ("SBUF arena overflow at", name, ar)
            ar["last"] = off
            ar["peak"] = max(ar.get("peak", 0), ar["lo"] + ARENA_BYTES - ar["hi"])
            return _view(off, shape, dt)

        ps = st.enter_context(nc.psum_tensor("ps", [128, 4096], F32))
        psb = ps[:].bitcast(BF16)
        rP = [Res("bank%d" % i) for i in range(8)]
        bk = lambda b, a=0, n=512: ps[:, b * 512 + a: b * 512 + a + n]
        ssl = lambda s0, n, stp: slice(s0, s0 + (n - 1) * stp + 1, stp)

        def MM(out, lhsT, rhs, start, stop, reads, writes):
            S.op("pe", lambda e: e.matmul(out, lhsT=lhsT, rhs=rhs, start=start, stop=stop,
                                          skip_group_check=True), reads, writes)

        def TR(out, in_, ident, reads, writes):
            S.op("pe", lambda e: e.transpose(out, in_, ident), reads, writes)

        def ACT(out, in_, func, reads, writes, scale=1.0, bias=None, accum_out=None):
            kw = {}
            if bias is not None:
                kw["bias"] = bias
            if accum_out is not None:
                kw["accum_out"] = accum_out
            S.op("act", lambda e: e.activation(out=out, in_=in_, func=func, scale=scale, **kw), reads, writes)

        def TT(out, in0, in1, op, reads, writes, eng="dve"):
            S.op(eng, lambda e: e.tensor_tensor(out=out, in0=in0, in1=in1, op=op), reads, writes)

        def TS(out, in0, s1, s2, op0, op1, reads, writes, eng="dve", accum_out=None):
            if accum_out is None:
                S.op(eng, lambda e: e.tensor_scalar(out=out, in0=in0, scalar1=s1, scalar2=s2, op0=op0, op1=op1),
                     reads, writes)
            else:
                S.op(eng, lambda e: e.tensor_scalar(out=out, in0=in0, scalar1=s1, scalar2=s2, op0=op0, op1=op1,
                                                    accum_out=accum_out), reads, writes)

        def STT(out, in0, scalar, in1, op0, op1, reads, writes, eng="dve"):
            S.op(eng, lambda e: e.scalar_tensor_tensor(out=out, in0=in0, scalar=scalar, in1=in1, op0=op0, op1=op1),
                 reads, writes)

        def CP(out, in_, reads, writes, eng="dve"):
            if eng == "act":
                S.op("act", lambda e: e.copy(out=out, in_=in_), reads, writes)
            else:
                S.op(eng, lambda e: e.tensor_copy(out=out, in_=in_), reads, writes)

        def DMA(eng, out, in_, reads, writes, dres):
            return S.op(eng, lambda e: e.dma_start(out=out, in_=in_), reads, writes, dma=dres)

        r_cv = S.dres("cv")
        cv_list = []
        for ex_ in range(NE):
            todo = [(wg_d, wgc, D)] + ([(wd_d, wdc, 1024)] if ex_ < NCV else [])
            for (src, dst, rows) in todo:
                hr = rows // 2
                for hh in range(2):
                    cv_list.append((dst[ex_, hh * hr:(hh + 1) * hr, :], src[ex_, hh * hr:(hh + 1) * hr, :]))
        cv_pos = [0]

        def emit_cv(n):
            for _ in range(n):
                if cv_pos[0] >= len(cv_list):
                    return
                o_, i_ = cv_list[cv_pos[0]]
                cv_pos[0] += 1
                DMA("pool", o_, i_, (), [r_cv], r_cv)

        identb = sb(st, "identb", [128, 128], BF16)
        identf = sb(st, "identf", [128, 128], F32)
        ustrict = sb(st, "ustrict", [128, 128], BF16)
        onesb = sb(st, "onesb", [128, 128], BF16)
        onesf = sb(st, "onesf", [128, 128], F32)
        blockones = sb(st, "blockones", [128, 128], BF16)
        iota_row = sb(st, "iota_row", [128, 128], F32)
        ebase = sb(st, "ebase", [128, 32], F32)
        tmpc = sb(st, "tmpc", [128, 128], F32, at=0)
        tmpc2 = sb(st, "tmpc2", [128, 128], F32, at=512)
        epsb = sb(st, "epsb", [128, 1], F32)
        r_const = Res("const")
        r_tmpc = Res("tmpc")
        r_tmpc2 = Res("tmpc2")
        S.op("pool", lambda e: e.iota(tmpc[:], pattern=[[1, 128]], base=0, channel_multiplier=-1,
                                      allow_small_or_imprecise_dtypes=True), (), [r_tmpc])
        S.op("dve", lambda e: e.tensor_single_scalar(out=identb[:], in_=tmpc[:], scalar=0.0, op=ALU.is_equal),
             [r_tmpc], [r_const])
        S.op("dve", lambda e: e.tensor_single_scalar(out=identf[:], in_=tmpc[:], scalar=0.0, op=ALU.is_equal),
             [r_tmpc], [r_const])
        S.op("dve", lambda e: e.tensor_single_scalar(out=ustrict[:], in_=tmpc[:], scalar=0.0, op=ALU.is_gt),
             [r_tmpc], [r_const])
        S.op("dve", lambda e: e.memset(onesb[:], 1.0), (), [r_const])
        S.op("dve", lambda e: e.memset(onesf[:], 1.0), (), [r_const])
        S.op("dve", lambda e: e.memset(epsb[:], EPS), (), [r_const])
        S.op("pool", lambda e: e.iota(iota_row[:], pattern=[[1, 128]], base=0, channel_multiplier=0,
                                      allow_small_or_imprecise_dtypes=True), (), [r_const])
        S.op("pool", lambda e: e.iota(tmpc2[:], pattern=[[0, 128]], base=0, channel_multiplier=1,
                                      allow_small_or_imprecise_dtypes=True), (), [r_tmpc2])
        S.op("dve", lambda e: e.tensor_single_scalar(out=tmpc2[:], in_=tmpc2[:], scalar=64.0, op=ALU.is_ge),
             [r_tmpc2], [r_tmpc2])
        S.op("dve", lambda e: e.tensor_single_scalar(out=tmpc[:], in_=iota_row[:], scalar=64.0, op=ALU.is_ge),
             [r_const, r_tmpc], [r_tmpc])
        TT(blockones[:], tmpc[:], tmpc2[:], ALU.is_equal, [r_tmpc, r_tmpc2], [r_const])
        S.op("pool", lambda e: e.iota(ebase[:], pattern=[[CAP, 32]], base=0, channel_multiplier=0,
                                      allow_small_or_imprecise_dtypes=True), (), [r_const])

        pstage = sb(st, "pstage", [48, 128], F32, at=1024)
        prm = sb(st, "prm", [128, 48], F32)
        cwst = sb(st, "cwst", [31, 1024], F32, at=1536)
        cw = sb(st, "cw", [128, 8, 31], F32)
        r_pstage = S.dres("pstage")
        r_cwst = S.dres("cwst")
        r_prm = Res("prm")
        r_cw = Res("cw")
        DMA("sync", pstage[:], pst, (), [r_pstage], r_pstage)
        DMA("sync", cwst[:], convw, (), [r_cwst], r_cwst)
        TR(bk(0, 0, 48), pstage[:], identf[0:48, 0:48], [r_pstage, r_const], [rP[0]])
        CP(prm[:], bk(0, 0, 48), [rP[0]], [r_prm])
        for j in range(8):
            TR(bk(1, j * 32, 31), cwst[:, j * 128:(j + 1) * 128], identf[0:31, 0:31], [r_cwst, r_const], [rP[1]])
        CP(cw[:], bk(1, 0, 256).rearrange("p (j k) -> p j k", j=8)[:, :, 0:31], [rP[1]], [r_cw])

        convT = sb(st, "convT", [128, 8, TOK], BF16)
        attnT = sb(st, "attnT", [128, 8, TOK], BF16)
        attn_off = ar["last"]
        r_convT = [Res("convT%d" % j) for j in range(8)]
        r_attnT = [Res("attnT%d" % j) for j in range(8)]

        S.barrier()
        with Scope() as s1:
            xnT = sb(s1, "xnT", [128, 16, NT], BF16)
            r_xnT = [Res("xnT%d" % t) for t in range(24)]
            wbuf = [sb(s1, "wbuf%d" % i, [128, 16, 128], BF16) for i in range(4)]
            r_wbuf = [S.dres("wbuf%d" % i) for i in range(4)]
            w_in_v = w_in.rearrange("(kc p) n -> p kc n", p=128)
            wslot = [0]

            def load_w(col0):
                i = wslot[0] % 4
                wslot[0] += 1
                DMA("pool", wbuf[i][:], w_in_v[:, :, col0:col0 + 128], (), [r_wbuf[i]], r_wbuf[i])
                return i

            with Scope() as sa:
                xt = [sb(sa, "xt%d" % i, [128, D], F32) for i in range(2)]
                r_xt = [S.dres("xt%d" % i) for i in range(2)]
                xn = [sb(sa, "xn%d" % i, [128, D], BF16) for i in range(2)]
                r_xn = [Res("xn%d" % i) for i in range(2)]
                g1bc = sb(sa, "g1bc", [128, D], F32)
                r_g1 = S.dres("g1bc")
                junk = sb(sa, "junk", [128, D], BF16)
                r_junk = Res("junk")
                ssq = sb(sa, "ssq", [128, 24], F32)
                rstd = sb(sa, "rstd", [128, 24], F32)
                r_ss = [Res("ss%d" % t) for t in range(24)]
                DMA("sync", g1bc[:], g1.broadcast_to([128, D]), (), [r_g1], r_g1)
                for tt in range(24):
                    b = tt % 2
                    DMA("sync", xt[b][:], xh[tt * 128:(tt + 1) * 128, :], (), [r_xt[b]], r_xt[b])
                    if tt % 4 == 3:
                        emit_cv(1)
                    ACT(junk[:], xt[b][:], AF.Square, [r_xt[b]], [r_junk, r_ss[tt]], accum_out=ssq[:, tt:tt + 1])
                    ACT(rstd[:, tt:tt + 1], ssq[:, tt:tt + 1], AF.Ln, [r_ss[tt], r_const], [r_ss[tt]],
                        scale=1.0 / D, bias=epsb[:, 0:1])
                    ACT(rstd[:, tt:tt + 1], rstd[:, tt:tt + 1], AF.Exp, [r_ss[tt]], [r_ss[tt]], scale=-0.5)
                    STT(xn[b][:], xt[b][:], rstd[:, tt:tt + 1], g1bc[:], ALU.mult, ALU.mult,
                        [r_xt[b], r_ss[tt], r_g1], [r_xn[b]])
                    for half in range(2):
                        bank = (2 * tt + half) % 4
                        for j in range(8):
                            kc = half * 8 + j
                            TR(psb[:, bank * 1024 + j * 128: bank * 1024 + (j + 1) * 128],
                               xn[b][:, kc * 128:(kc + 1) * 128], identb[:], [r_xn[b], r_const], [rP[bank]])
                        CP(xnT[:, half * 8:(half + 1) * 8, tt * 128:(tt + 1) * 128],
                           psb[:, bank * 1024:(bank + 1) * 1024].rearrange("p (j t) -> p j t", j=8),
                           [rP[bank]], [r_xnT[tt]], eng=("act" if half == 0 else "dve"))
                S.barrier()

            with Scope() as sc:
                NU = 1152
                ub = [sb(sc, "u%d" % i, [128, NU], F32) for i in range(2)]
                r_u = [Res("u%d" % i) for i in range(2)]
                sig = [sb(sc, "sig%d" % i, [128, 512], F32) for i in range(2)]
                r_sig = [Res("sig%d" % i) for i in range(2)]
                craw = sb(sc, "craw", [128, 8, TOK], F32)
                r_craw = [Res("craw%d" % j) for j in range(8)]
                csq = sb(sc, "csq", [128, 512], F32)
                r_csq = Res("csq")
                mean = sb(sc, "mean", [128, TOK], F32, at=attn_off)
                lrs = sb(sc, "lrs", [128, TOK], F32, at=attn_off + 4096)
                r_mean = Res("mean")
                r_lrs = Res("lrs")
                tln = [sb(sc, "tln%d" % i, [128, TOK], F32, at=attn_off + 8192) for i in range(1)]
                r_tln = [Res("tln%d" % i) for i in range(1)]
                blocks = [(1920, 512), (2432, 512), (2944, 128)]
                pbank = [0]

                def nb():
                    pbank[0] = (pbank[0] + 1) % 8
                    return pbank[0]

                r_crawh = [[Res("craw%d_%d" % (j, hf)) for hf in range(2)] for j in range(8)]
                wpre = {}
                for j in range(2):
                    wpre[j] = (load_w(j * 128), load_w(1024 + j * 128))
                for j in range(8):
                    wv, wg = wpre[j]
                    u = ub[j % 2]
                    ru = r_u[j % 2]
                    for bi, (t0, n) in enumerate(blocks):
                        bv, bg = nb(), nb()
                        for kc in range(16):
                            MM(bk(bv, 0, n), wbuf[wv][:, kc, :], xnT[:, kc, t0:t0 + n], kc == 0, kc == 15,
                               [r_wbuf[wv]] + r_xnT[t0 // 128:(t0 + n) // 128], [rP[bv]])
                        for kc in range(16):
                            MM(bk(bg, 0, n), wbuf[wg][:, kc, :], xnT[:, kc, t0:t0 + n], kc == 0, kc == 15,
                               [r_wbuf[wg]] + r_xnT[t0 // 128:(t0 + n) // 128], [rP[bg]])
                        sg = sig[bi % 2]
                        ACT(sg[:, 0:n], bk(bg, 0, n), AF.Sigmoid, [rP[bg]], [r_sig[bi % 2]])
                        TT(u[:, t0 - 1920:t0 - 1920 + n], bk(bv, 0, n), sg[:, 0:n], ALU.mult,
                           [rP[bv], r_sig[bi % 2]], [ru])
                    if j + 2 < 8:
                        wpre[j + 2] = (load_w((j + 2) * 128), load_w(1024 + (j + 2) * 128))
                    emit_cv(3)
                    for k in range(31):
                        for hf in range(2):
                            acc = craw[:, j, hf * 512:(hf + 1) * 512]
                            usl = u[:, 98 + k + hf * 512:98 + k + hf * 512 + 512]
                            rc = r_crawh[j][hf]
                            if k == 0:
                                TS(acc, usl, cw[:, j, 0:1], prm[:, j:j + 1], ALU.mult, ALU.add, [ru, r_cw, r_prm], [rc])
                            else:
                                STT(acc, usl, cw[:, j, k:k + 1], acc, ALU.mult, ALU.add, [ru, r_cw, rc], [rc])
                for blk in range(2):
                    c0 = blk * 512
                    bs, bq = nb(), nb()
                    for j in range(8):
                        MM(bk(bs), onesf[:], craw[:, j, c0:c0 + 512], j == 0, j == 7, [r_const] + r_crawh[j], [rP[bs]])
                    for j in range(8):
                        ACT(csq[:, 0:512], craw[:, j, c0:c0 + 512], AF.Square, r_crawh[j], [r_csq])
                        MM(bk(bq), onesf[:], csq[:, 0:512], j == 0, j == 7, [r_const, r_csq], [rP[bq]])
                    ACT(mean[:, c0:c0 + 512], bk(bs), AF.Copy, [rP[bs]], [r_mean], scale=1.0 / 1024)
                    TT(lrs[:, c0:c0 + 512], mean[:, c0:c0 + 512], mean[:, c0:c0 + 512], ALU.mult, [r_mean], [r_lrs])
                    STT(lrs[:, c0:c0 + 512], bk(bq), 1.0 / 1024, lrs[:, c0:c0 + 512], ALU.mult, ALU.subtract,
                        [rP[bq], r_lrs], [r_lrs])
                    ACT(lrs[:, c0:c0 + 512], lrs[:, c0:c0 + 512], AF.Ln, [r_lrs, r_const], [r_lrs], bias=epsb[:, 0:1])
                    ACT(lrs[:, c0:c0 + 512], lrs[:, c0:c0 + 512], AF.Exp, [r_lrs], [r_lrs], scale=-0.5)
                for j in range(8):
                    t = tln[0]
                    rt = r_tln[0]
                    TT(t[:], craw[:, j, :], mean[:], ALU.subtract, r_crawh[j] + [r_mean], [rt])
                    TT(t[:], t[:], lrs[:], ALU.mult, [rt, r_lrs], [rt])
                    ACT(convT[:, j, :], t[:], AF.Silu, [rt, r_prm], [r_convT[j]],
                        scale=prm[:, 8 + j:9 + j], bias=prm[:, 16 + j:17 + j])
                S.barrier()

            with Scope() as sat:
                qT = sb(sat, "qT", [128, TOK], BF16)
                kT = sb(sat, "kT", [128, NT], BF16)
                vT = sb(sat, "vT", [128, NT], BF16)
                r_qT = [Res("qT%d" % i) for i in range(2)]
                r_kT = [Res("kT%d" % i) for i in range(6)]
                r_vT = [Res("vT%d" % i) for i in range(6)]
                vt = sb(sat, "vt", [128, NKT, 2, 65], BF16)
                r_vt = [Res("vt%d" % i) for i in range(NKT)]
                vcol = sb(sat, "vcol", [128, NKT], F32)
                r_vcol = S.dres("vcol")
                bm = sb(sat, "bmt", [128, 1536], F32)
                r_bm = S.dres("bm")
                sqb = [sb(sat, "sqb%d" % i, [128, 512], BF16) for i in range(2)]
                r_sqb = [Res("sqb%d" % i) for i in range(2)]
                rs = [sb(sat, "rs%d" % i, [128, 512], F32) for i in range(2)]
                r_rs = [Res("rs%d" % i) for i in range(2)]
                tsc = [sb(sat, "tsc%d" % i, [128, 512], F32) for i in range(2)]
                r_tsc = [Res("tsc%d" % i) for i in range(2)]
                pt = [sb(sat, "pt%d" % i, [128, 512], BF16) for i in range(3)]
                r_pt = [Res("pt%d" % i) for i in range(3)]
                osb = sb(sat, "osb", [65, TOK], F32)
                r_osb = Res("osb")
                rden = sb(sat, "rden", [128, 512], F32)
                r_rden = Res("rden")
                selN = sb(sat, "selN", [65, 2, 128], F32)
                selD = sb(sat, "selD", [65, 2, 128], F32)
                r_sel = Res("sel")
                S.op("dve", lambda e: e.memset(selN[:], 0.0), (), [r_sel])
                S.op("dve", lambda e: e.memset(selD[:], 0.0), (), [r_sel])
                CP(selN[0:64, 0, 0:64], identf[0:64, 0:64], [r_const, r_sel], [r_sel])
                CP(selN[0:64, 1, 64:128], identf[0:64, 0:64], [r_const, r_sel], [r_sel])
                S.op("dve", lambda e: e.memset(selD[64:65, 0, 0:64], 1.0), [r_sel], [r_sel])
                S.op("dve", lambda e: e.memset(selD[64:65, 1, 64:128], 1.0), [r_sel], [r_sel])
                DMA("sync", vcol[:], vcol_d, (), [r_vcol], r_vcol)
                for h in range(2):
                    CP(vt[:, :, h, 64], vcol[:], [r_vcol], r_vt)
                scale = 1.0 / 8.0
                BP = (4, 5, 0, 1)
                BS = (2, 3)
                BN = 6
                BT = 7
                pcount = [0]
                scount = [0]
                pcnt = [0]

                def qk_proj(wi, dst, rdst, t0, hp, gcol):
                    b = BP[pcount[0] % 4]
                    i2 = pcount[0] % 2
                    pcount[0] += 1
                    for kc in range(16):
                        MM(bk(b), wbuf[wi][:, kc, :], xnT[:, kc, t0:t0 + 512], kc == 0, kc == 15,
                           [r_wbuf[wi]] + r_xnT[t0 // 128:t0 // 128 + 4], [rP[b]])
                    ACT(sqb[i2][:], bk(b), AF.Square, [rP[b]], [r_sqb[i2]])

                    def part2():
                        MM(bk(BN), blockones[:], sqb[i2][:], True, True, [r_const, r_sqb[i2]], [rP[BN]])
                        ACT(rs[i2][:], bk(BN), AF.Ln, [rP[BN], r_const], [r_rs[i2]], scale=1.0 / 64,
                            bias=epsb[:, 0:1])
                        ACT(rs[i2][:], rs[i2][:], AF.Exp, [r_rs[i2]], [r_rs[i2]], scale=-0.5)
                        STT(dst, bk(b), prm[:, gcol:gcol + 1], rs[i2][:], ALU.mult, ALU.mult,
                            [rP[b], r_prm, r_rs[i2]], [rdst])
                    return part2

                for hp in range(8):
                    wq = load_w(2048 + hp * 128)
                    wk = load_w(3072 + hp * 128)
                    wv_ = load_w(4096 + hp * 128)
                    emit_cv(7)
                    pend = None
                    for i in range(2):
                        p2 = qk_proj(wq, qT[:, i * 512:(i + 1) * 512], r_qT[i], 2048 + i * 512, hp, 24)
                        if pend is not None:
                            pend()
                        pend = p2
                    for i in range(6):
                        p2 = qk_proj(wk, kT[:, i * 512:(i + 1) * 512], r_kT[i], i * 512, hp, 25)
                        if pend is not None:
                            pend()
                        pend = p2
                    for i in range(6):
                        b = BP[pcount[0] % 4]
                        pcount[0] += 1
                        t0 = i * 512
                        for kc in range(16):
                            MM(bk(b), wbuf[wv_][:, kc, :], xnT[:, kc, t0:t0 + 512], kc == 0, kc == 15,
                               [r_wbuf[wv_]] + r_xnT[t0 // 128:t0 // 128 + 4], [rP[b]])
                        CP(vT[:, t0:t0 + 512], bk(b), [rP[b]], [r_vT[i]], eng="act")
                        if pend is not None:
                            pend()
                            pend = None
                    for g0 in range(0, NKT, 8):
                        n = min(8, NKT - g0)
                        for ti in range(n):
                            s0, stp = KT[g0 + ti]
                            TR(psb[:, BT * 1024 + ti * 128: BT * 1024 + (ti + 1) * 128],
                               vT[:, ssl(s0, 128, stp)], identb[:], r_vT + [r_const], [rP[BT]])
                        CP(vt[:, g0:g0 + n, :, 0:64],
                           psb[:, BT * 1024: BT * 1024 + n * 128].rearrange("p (t h d) -> p t h d", t=n, h=2),
                           [rP[BT]], r_vt[g0:g0 + n], eng="dve")
                    for h in range(2):
                        hs = slice(h * 64, (h + 1) * 64)
                        DMA("sync", bm[:], bm_d[2 * hp + h], (), [r_bm], r_bm)
                        first = [True, True]
                        pv_pend = [None]

                        def score_group(items, bmcol, h=h, hs=hs):
                            b = BS[scount[0] % 2]
                            i2 = scount[0] % 2
                            ip = pcnt[0] % 3
                            scount[0] += 1
                            pcnt[0] += 1
                            for (kt, (q0, qn, qs), off) in items:
                                s0, stp = KT[kt]
                                MM(bk(b, off, qn), kT[hs, ssl(s0, 128, stp)], qT[hs, ssl(q0, qn, qs)],
                                   True, True, r_kT + r_qT, [rP[b]])
                            STT(tsc[i2][:], bk(b), scale, bm[:, bmcol:bmcol + 512], ALU.mult, ALU.add,
                                [rP[b], r_bm], [r_tsc[i2]])
                            ACT(pt[ip][:], tsc[i2][:], AF.Exp, [r_tsc[i2]], [r_pt[ip]])
                            prev = pv_pend[0]
                            pv_pend[0] = lambda: pv_part(items, ip, h)
                            if prev is not None:
                                prev()

                        def pv_part(items, ip, h):
                            for (kt, (q0, qn, qs), off) in items:
                                segs = []
                                if q0 + (qn - 1) * qs < 512 or q0 >= 512:
                                    segs.append((0, qn))
                                else:
                                    n0 = (512 - q0 + qs - 1) // qs
                                    segs.append((0, n0))
                                    segs.append((n0, qn - n0))
                                for (i0, nn) in segs:
                                    c0 = q0 + i0 * qs
                                    ob = c0 // 512
                                    cc = c0 % 512
                                    MM(ps[0:65, ob * 512 + cc: ob * 512 + cc + (nn - 1) * qs + 1: qs],
                                       vt[:, kt, h, :], pt[ip][:, off + i0: off + i0 + nn],
                                       first[ob], False, [r_vt[kt], r_pt[ip]], [rP[ob]])
                                    first[ob] = False

                        for b2 in range(0, 8, 2):
                            items = []
                            for x in range(2):
                                qb = b2 + x
                                items.append((qb, (qb * 128, 128, 1), x * 256))
                                items.append((qb + 1, (qb * 128, 128, 1), x * 256 + 128))
                            score_group(items, 0)
                        for r in range(4):
                            items = []
                            for x in range(2):
                                items.append((9 + 3 * r + x, (512 * x + r, 128, 4), x * 256))
                                items.append((9 + 3 * r + x + 1, (512 * x + r, 128, 4), x * 256 + 128))
                            score_group(items, 512)
                        for r4 in range(0, 16, 4):
                            items = []
                            for x in range(4):
                                r = r4 + x
                                items.append((21 + 2 * r, (r, 64, 16), x * 128))
                                items.append((21 + 2 * r + 1, (r, 64, 16), x * 128 + 64))
                            score_group(items, 1024)
                        pv_pend[0]()
                        pv_pend[0] = None
                        CP(osb[:, :], ps[0:65, 0:1024], [rP[0], rP[1]], [r_osb], eng="act")
                        for blk in range(2):
                            c0 = blk * 512
                            bN = (4, 5)[pcount[0] % 2]
                            pcount[0] += 1
                            MM(bk(BT), selD[:, h, :], osb[:, c0:c0 + 512], True, True, [r_sel, r_osb], [rP[BT]])
                            MM(bk(bN), selN[:, h, :], osb[:, c0:c0 + 512], True, True, [r_sel, r_osb], [rP[bN]])
                            S.op("dve", lambda e, c0=c0, hs=hs: e.reciprocal(out=rden[hs, :], in_=bk(BT)[hs, :]),
                                 [rP[BT]], [r_rden])
                            TT(attnT[hs, hp, c0:c0 + 512], bk(bN)[hs, :], rden[hs, :], ALU.mult,
                               [rP[bN], r_rden], [r_attnT[hp]])
                S.barrier()

        with Scope() as s2:
            hn = sb(s2, "hn", [128, 8, D], BF16)
            r_hn = [Res("hn%d" % i) for i in range(8)]
            oh1 = sb(s2, "oh1", [128, 8, 32], F32)
            oh2 = sb(s2, "oh2", [128, 8, 32], F32)
            m32 = sb(s2, "m32", [128, 8, 32], BF16)
            g32 = sb(s2, "g32", [128, 8, 32], F32)
            rank = sb(s2, "rank", [128, 8, 32], F32)
            rankm = sb(s2, "rankm", [128, 8, 32], F32)
            m32f = sb(s2, "m32f", [128, 8, 32], F32)
            r_rt = [Res("route%d" % i) for i in range(8)]
            r_rank = Res("rank")
            idx = sb(s2, "idx", [128, 8, 2], I32)
            gsel = sb(s2, "gsel", [128, 8, 2], F32)
            r_idx = Res("idx")

            with Scope() as so:
                wo = sb(so, "wo", [128, 16, D], BF16)
                r_wo = [S.dres("wo%d" % i) for i in range(4)]
                w_out_v = w_out.rearrange("(kc p) n -> p kc n", p=128)
                for i in range(4):
                    DMA("pool", wo[:, :, i * 512:(i + 1) * 512], w_out_v[:, :, i * 512:(i + 1) * 512], (),
                        [r_wo[i]], r_wo[i])
                emit_cv(len(cv_list))
                g2bc = sb(so, "g2bc", [128, D], F32)
                r_g2 = S.dres("g2bc")
                DMA("sync", g2bc[:], g2.broadcast_to([128, D]), (), [r_g2], r_g2)
                wr = sb(so, "wr", [128, 16, 36], F32)
                r_wr = S.dres("wr")
                DMA("sync", wr[:], wr_d.rearrange("(kc p) n -> p kc n", p=128), (), [r_wr], r_wr)
                for kc in range(16):
                    TS(wr[:, kc, :], wr[:, kc, :], prm[:, 26 + kc:27 + kc], None, ALU.mult, ALU.bypass,
                       [r_wr, r_prm], [r_wr])
                brbc = sb(so, "brbc", [128, 36], F32)
                r_br = S.dres("brbc")
                DMA("sync", brbc[:], br_d.broadcast_to([128, 36]), (), [r_br], r_br)
                xr = [sb(so, "xr%d" % i, [128, D], F32) for i in range(2)]
                r_xr = [S.dres("xr%d" % i) for i in range(2)]
                x1 = [sb(so, "x1_%d" % i, [128, D], F32) for i in range(2)]
                r_x1 = [S.dres("x1_%d" % i) for i in range(2)]
                junk2 = sb(so, "junk2", [128, D], BF16)
                r_junk2 = Res("junk2")
                x1T = sb(so, "x1T", [128, 16, 128], F32)
                r_x1T = Res("x1T")
                st2 = sb(so, "st2", [128, 8, 4], F32)
                lg = sb(so, "lg", [128, 36], F32)
                r_lg = Res("lg")
                sc8 = sb(so, "sc8", [128, 64], F32)
                r_sc = Res("sc8")
                mixT = lambda c: (convT[:, c, :] if c < 8 else attnT[:, c - 8, :])
                r_mix = r_convT + r_attnT
                r_out_rows = [Res("outrows%d" % i) for i in range(8)]
                for tt in range(8):
                    b = tt % 2
                    DMA("sync", xr[b][:], xh[HALO + tt * 128: HALO + (tt + 1) * 128, :], (), [r_xr[b]], r_xr[b])
                    for cb in range(4):
                        for c in range(16):
                            MM(bk(cb), mixT(c)[:, tt * 128:(tt + 1) * 128], wo[:, c, cb * 512:(cb + 1) * 512],
                               c == 0, c == 15, [r_mix[c], r_wo[cb]], [rP[cb]])
                    TT(x1[b][:], ps[:, 0:2048], xr[b][:], ALU.add, rP[0:4] + [r_xr[b]], [r_x1[b]])
                    DMA("sync", out_d[tt * 128:(tt + 1) * 128, :], x1[b][:], [r_x1[b]], [r_out_rows[tt]], r_x1[b])
                    ACT(junk2[:], x1[b][:], AF.Square, [r_x1[b]], [r_junk2, r_sc], accum_out=st2[:, tt, 0:1])
                    ACT(st2[:, tt, 1:2], st2[:, tt, 0:1], AF.Ln, [r_sc, r_const], [r_sc], scale=1.0 / D,
                        bias=epsb[:, 0:1])
                    ACT(st2[:, tt, 1:2], st2[:, tt, 1:2], AF.Exp, [r_sc], [r_sc], scale=-0.5)
                    STT(hn[:, tt, :], x1[b][:], st2[:, tt, 1:2], g2bc[:], ALU.mult, ALU.mult,
                        [r_x1[b], r_sc, r_g2], [r_hn[tt]])
                    for half in range(2):
                        for j in range(8):
                            kc = half * 8 + j
                            TR(ps[:, (4 + half * 2) * 512 + j * 128:(4 + half * 2) * 512 + (j + 1) * 128],
                               x1[b][:, kc * 128:(kc + 1) * 128], identf[:], [r_x1[b], r_const],
                               [rP[4 + half * 2], rP[5 + half * 2]])
                        CP(x1T[:, half * 8:(half + 1) * 8, :],
                           ps[:, (4 + half * 2) * 512:(6 + half * 2) * 512].rearrange("p (j t) -> p j t", j=8),
                           [rP[4 + half * 2], rP[5 + half * 2]], [r_x1T], eng=("act" if half == 0 else "dve"))
                    for kc in range(16):
                        MM(bk(4, 0, 36), x1T[:, kc, :], wr[:, kc, :], kc == 0, kc == 15, [r_x1T, r_wr], [rP[4]])
                    STT(lg[:], bk(4, 0, 36), st2[:, tt, 1:2], brbc[:], ALU.mult, ALU.add, [rP[4], r_sc, r_br], [r_lg])
                    R = [r_lg, r_sc, r_rt[tt]]
                    W = [r_sc, r_rt[tt]]
                    gmax = sc8[:, 0:1]
                    S.op("dve", lambda e: e.tensor_reduce(out=sc8[:, 0:1], in_=lg[:, 0:4], axis=AX.X, op=ALU.max), R, W)
                    TS(sc8[:, 4:8], lg[:, 0:4], sc8[:, 0:1], None, ALU.is_equal, ALU.bypass, R, W)
                    TS(sc8[:, 1:2], sc8[:, 0:1], -1.0, None, ALU.mult, ALU.bypass, R, W)
                    ACT(sc8[:, 8:12], lg[:, 0:4], AF.Exp, R, W, bias=sc8[:, 1:2])
                    S.op("dve", lambda e: e.tensor_reduce(out=sc8[:, 2:3], in_=sc8[:, 8:12], axis=AX.X, op=ALU.add), R, W)
                    S.op("dve", lambda e: e.reciprocal(out=sc8[:, 3:4], in_=sc8[:, 2:3]), R, W)
                    TS(sc8[:, 16:24], lg[:, 4:12], sc8[:, 4:5], None, ALU.mult, ALU.bypass, R, W)
                    for g in range(1, 4):
                        STT(sc8[:, 16:24], lg[:, 4 + 8 * g:12 + 8 * g], sc8[:, 4 + g:5 + g], sc8[:, 16:24],
                            ALU.mult, ALU.add, R, W)
                    S.op("dve", lambda e: e.max(out=sc8[:, 24:32], in_=sc8[:, 16:24]), R, W)
                    TS(sc8[:, 32:40], sc8[:, 16:24], sc8[:, 24:25], None, ALU.is_equal, ALU.bypass, R, W)
                    TS(sc8[:, 40:48], sc8[:, 16:24], sc8[:, 25:26], None, ALU.is_equal, ALU.bypass, R, W)
                    TT(sc8[:, 12:13], sc8[:, 24:25], sc8[:, 25:26], ALU.subtract, R, W)
                    ACT(sc8[:, 12:13], sc8[:, 12:13], AF.Exp, R, W)
                    TS(sc8[:, 12:13], sc8[:, 12:13], 1.0, None, ALU.add, ALU.bypass, R, W)
                    S.op("dve", lambda e: e.reciprocal(out=sc8[:, 13:14], in_=sc8[:, 12:13]), R, W)
                    TS(sc8[:, 14:15], sc8[:, 13:14], -1.0, 1.0, ALU.mult, ALU.add, R, W)
                    TT(sc8[:, 13:15], sc8[:, 13:15], sc8[:, 3:4].to_broadcast([128, 2]), ALU.mult, R, W)
                    for g in range(4):
                        TS(oh1[:, tt, 8 * g:8 * g + 8], sc8[:, 32:40], sc8[:, 4 + g:5 + g], None, ALU.mult, ALU.bypass, R, W)
                        TS(oh2[:, tt, 8 * g:8 * g + 8], sc8[:, 40:48], sc8[:, 4 + g:5 + g], None, ALU.mult, ALU.bypass, R, W)
                    TT(m32[:, tt, :], oh1[:, tt, :], oh2[:, tt, :], ALU.add, R, W)
                    TT(m32f[:, tt, :], oh1[:, tt, :], oh2[:, tt, :], ALU.add, R, W)
                    TS(g32[:, tt, :], oh1[:, tt, :], sc8[:, 14:15], None, ALU.mult, ALU.bypass, R, W)
                    STT(g32[:, tt, :], oh2[:, tt, :], sc8[:, 13:14], g32[:, tt, :], ALU.mult, ALU.add, R, W)
                for tt in range(8):
                    for j in range(tt + 1):
                        MM(bk(5, tt * 32, 32), (ustrict[:] if j == tt else onesb[:]), m32[:, j, :], j == 0, j == tt,
                           [r_const, r_rt[j]], [rP[5]])
                CP(rank[:], bk(5, 0, 256).rearrange("p (t e) -> p t e", t=8), [rP[5]], [r_rank])
                STT(rankm[:], rank[:], 1.0, m32f[:], ALU.add, ALU.mult, r_rt + [r_rank], [r_rank])
                TS(rankm[:], rankm[:], -1.0, None, ALU.add, ALU.bypass, [r_rank], [r_rank])
                sl = sb(so, "sl", [128, 8, 32], F32)
                ok = sb(so, "ok", [128, 8, 32], F32)
                red = sb(so, "red", [128, 8, 4], F32)
                r_sl = Res("sl")
                Rr = r_rt + [r_rank, r_sl, r_const]
                Wr_ = [r_sl]
                TT(sl[:], rank[:], ebase[:].unsqueeze(1).to_broadcast([128, 8, 32]), ALU.add, Rr, Wr_)
                TS(ok[:], rank[:], float(CAP), None, ALU.is_lt, ALU.bypass, Rr, Wr_)
                STT(sl[:], sl[:], -float(ZROW), ok[:], ALU.add, ALU.mult, Rr, Wr_)
                TS(sl[:], sl[:], float(ZROW), None, ALU.add, ALU.bypass, Rr, Wr_)
                for k, oh in enumerate((oh1, oh2)):
                    TT(ok[:], sl[:], oh[:], ALU.mult, Rr, Wr_)
                    S.op("dve", lambda e, k=k: e.tensor_reduce(out=red[:, :, k], in_=ok[:], axis=AX.X, op=ALU.add), Rr, Wr_)
                    TT(ok[:], g32[:], oh[:], ALU.mult, Rr, Wr_)
                    S.op("dve", lambda e, k=k: e.tensor_reduce(out=gsel[:, :, k], in_=ok[:], axis=AX.X, op=ALU.add),
                         Rr, Wr_ + [r_idx])
                CP(idx[:], red[:, :, 0:2], Rr, [r_idx])
                S.barrier()

            if STOP_AFTER == "mixer":
                S.finish([(r.dsem, r.dcount, "dma") for r in S.dsems if r.dcount > 0])
                S.replay()
                return nc

            with Scope() as sm:
                NSTG = 3
                wgb = [sb(sm, "wgb%d" % i, [128, 16, 256], BF16) for i in range(NSTG)]
                wub = [sb(sm, "wub%d" % i, [128, 16, 256], BF16) for i in range(NSTG)]
                wdb = [sb(sm, "wdb%d" % i, [128, 2, D], BF16) for i in range(NSTG)]
                r_wgb = [S.dres("wgb%d" % i) for i in range(NSTG)]
                r_wub = [S.dres("wub%d" % i) for i in range(NSTG)]
                r_wdb = [S.dres("wdb%d" % i) for i in range(NSTG)]
                se = [sb(sm, "se%d" % i, [128, 8, 128], BF16) for i in range(2)]
                r_se = [Res("se%d" % i) for i in range(2)]
                xe = [sb(sm, "xe%d" % i, [128, 16, 128], BF16) for i in range(2)]
                r_xe = [Res("xe%d" % i) for i in range(2)]
                sgt = [sb(sm, "sgt%d" % i, [128, 256], F32) for i in range(2)]
                r_sgt = [Res("sgt%d" % i) for i in range(2)]
                hT = [sb(sm, "hT%d" % i, [128, 2, 128], BF16) for i in range(2)]
                r_hT = [Res("hT%d" % i) for i in range(2)]
                yb = [sb(sm, "yb%d" % i, [128, D], F32) for i in range(2)]
                r_yb = [S.dres("yb%d" % i) for i in range(2)]
                r_yd = Res("yd")
                y1b = sb(sm, "y1b", [128, D], F32)
                y2b = sb(sm, "y2b", [128, D], F32)
                zt = y1b
                r_zt = S.dres("zt")
                S.op("dve", lambda e: e.memset(zt[:], 0.0), (), [r_zt])
                DMA("sync", yd[ZROW:ZROW + 128, :], zt[:], [r_zt], [r_yd], r_zt)
                stage = 0
                order = []
                ia, ib = 0, NCV
                while ia < NCV or ib < NE:
                    if ia < NCV:
                        order.append(ia)
                        ia += 1
                    if ib < NE:
                        order.append(ib)
                        ib += 1
                    if ib < NE and len(order) % 3 == 2:
                        order.append(ib)
                        ib += 1
                assert sorted(order) == list(range(NE))
                for ei, ex in enumerate(order):
                    e2 = ei % 2
                    for tt in range(8):
                        TS(se[e2][:, tt, :], iota_row[:], rankm[:, tt, ex:ex + 1], None,
                           ALU.is_equal, ALU.bypass, [r_const, r_rank, r_rt[tt]], [r_se[e2]])
                    for g4 in range(4):
                        gb = 6 + (g4 % 2)
                        for c in range(4):
                            kc = g4 * 4 + c
                            for tt in range(8):
                                MM(bk(gb, c * 128, 128), hn[:, tt, kc * 128:(kc + 1) * 128], se[e2][:, tt, :],
                                   tt == 0, tt == 7, [r_hn[tt], r_se[e2]], [rP[gb]])
                        CP(xe[e2][:, g4 * 4:(g4 + 1) * 4, :], bk(gb).rearrange("p (c s) -> p c s", c=4),
                           [rP[gb]], [r_xe[e2]], eng=("act" if g4 % 2 == 0 else "dve"))
                    for fq in range(4):
                        sgi = stage % NSTG
                        sg_, su_, sd_ = wgc, wu_d, (wdc if ex < NCV else wd_d)
                        rcv = [r_cv]
                        DMA("pool", wgb[sgi][:], sg_[ex].rearrange("(kc p) f -> p kc f", p=128)[:, :, fq * 256:(fq + 1) * 256],
                            rcv, [r_wgb[sgi]], r_wgb[sgi])
                        DMA("pool", wub[sgi][:], su_[ex].rearrange("(kc p) f -> p kc f", p=128)[:, :, fq * 256:(fq + 1) * 256],
                            rcv, [r_wub[sgi]], r_wub[sgi])
                        DMA("pool", wdb[sgi][:], sd_[ex, fq * 256:(fq + 1) * 256, :].rearrange("(fc p) d -> p fc d", p=128),
                            rcv, [r_wdb[sgi]], r_wdb[sgi])
                        hb = 4 + (stage % 2)
                        h2 = stage % 2
                        for fc in range(2):
                            for kc in range(16):
                                MM(bk(hb, fc * 128, 128), wgb[sgi][:, kc, fc * 128:(fc + 1) * 128], xe[e2][:, kc, :],
                                   kc == 0, kc == 15, [r_wgb[sgi], r_xe[e2]], [rP[hb]])
                            for kc in range(16):
                                MM(bk(hb, 256 + fc * 128, 128), wub[sgi][:, kc, fc * 128:(fc + 1) * 128], xe[e2][:, kc, :],
                                   kc == 0, kc == 15, [r_wub[sgi], r_xe[e2]], [rP[hb]])
                        ACT(sgt[h2][:], bk(hb, 0, 256), AF.Silu, [rP[hb]], [r_sgt[h2]])
                        TT(hT[h2][:].rearrange("p c s -> p (c s)"), sgt[h2][:], bk(hb, 256, 256), ALU.mult,
                           [r_sgt[h2], rP[hb]], [r_hT[h2]])
                        for fc in range(2):
                            for cb in range(4):
                                MM(bk(cb), hT[h2][:, fc, :], wdb[sgi][:, fc, cb * 512:(cb + 1) * 512],
                                   fq == 0 and fc == 0, fq == 3 and fc == 1, [r_hT[h2], r_wdb[sgi]], [rP[cb]])
                        stage += 1
                    CP(yb[e2][:], ps[:, 0:2048], rP[0:4], [r_yb[e2]], eng="act")
                    DMA("sync", yd[ex * CAP:(ex + 1) * CAP, :], yb[e2][:], [r_yb[e2]], [r_yd], r_yb[e2])
                with Scope() as sf:
                    y1 = [sb(sf, "y1a", [128, D], F32, at=attn_off + 16384), y1b]
                    y2 = [sb(sf, "y2a", [128, D], F32, at=attn_off + 24576), y2b]
                    xo = [sb(sf, "xo0", [128, D], F32, at=attn_off), sb(sf, "xo1", [128, D], F32, at=attn_off + 8192)]
                    r_y1 = [S.dres("y1_0"), r_zt]
                    r_y2 = [S.dres("y2_%d" % i) for i in range(2)]
                    r_xo = [S.dres("xo%d" % i) for i in range(2)]
                    fin = []
                    for tt in range(8):
                        b = tt % 2
                        DMA("sync", xo[b][:], out_d[tt * 128:(tt + 1) * 128, :], [r_out_rows[tt]], [r_xo[b]], r_xo[b])
                        S.op("pool", lambda e, tt=tt, b=b: e.indirect_dma_start(
                            out=y1[b][:], out_offset=None, in_=yd,
                            in_offset=bass.IndirectOffsetOnAxis(ap=idx[:, tt, 0:1], axis=0),
                            bounds_check=NE * CAP + 127, oob_is_err=False), [r_idx, r_yd], [r_y1[b]], dma=r_y1[b])
                        S.op("pool", lambda e, tt=tt, b=b: e.indirect_dma_start(
                            out=y2[b][:], out_offset=None, in_=yd,
                            in_offset=bass.IndirectOffsetOnAxis(ap=idx[:, tt, 1:2], axis=0),
                            bounds_check=NE * CAP + 127, oob_is_err=False), [r_idx, r_yd], [r_y2[b]], dma=r_y2[b])
                        STT(xo[b][:], y1[b][:], gsel[:, tt, 0:1], xo[b][:], ALU.mult, ALU.add,
                            [r_y1[b], r_idx, r_xo[b]], [r_xo[b]])
                        STT(xo[b][:], y2[b][:], gsel[:, tt, 1:2], xo[b][:], ALU.mult, ALU.add,
                            [r_y2[b], r_idx, r_xo[b]], [r_xo[b]])
                        fin.append(DMA("sync", out_d[tt * 128:(tt + 1) * 128, :], xo[b][:], [r_xo[b]],
                                       [r_out_rows[tt]], r_xo[b]))
                    S.finish(fin)
                    S.replay()
    return nc


_CACHE = {}


def prep_inputs(x, norm1_g, w_in, q_norm_g, k_norm_g, conv_w, conv_b, conv_ln_g, conv_ln_b,
                rel_bias, w_out, norm2_g, w_router_group, b_router_group, w_router_expert,
                b_router_expert, w_gate, w_up, w_down):
    f = lambda a: np.ascontiguousarray(np.asarray(a, dtype=np.float32))
    x = f(x)[0]
    xpad = np.concatenate([np.zeros((HALO, D), np.float32), x], axis=0)
    pst = np.zeros((48, 128), np.float32)
    pst[0:8] = f(conv_b)[0].reshape(8, 128)
    pst[8:16] = f(conv_ln_g)[0].reshape(8, 128)
    pst[16:24] = f(conv_ln_b)[0].reshape(8, 128)
    pst[24] = np.tile(f(q_norm_g)[0], 2)
    pst[25] = np.tile(f(k_norm_g)[0], 2)
    pst[26:42] = f(norm2_g)[0].reshape(16, 128)
    bidx, mask = _bucket_tables()
    rb = f(rel_bias)
    bm = np.where(mask[None], rb[bidx].transpose(2, 0, 1), np.float32(NEG)).astype(np.float32)
    wr = np.concatenate([f(w_router_group)[0]] + [f(w_router_expert)[0, g] for g in range(4)], axis=1)
    br = np.concatenate([f(b_router_group)[0], f(b_router_expert)[0].reshape(-1)])[None, :]
    shared = {
        "w_in": f(w_in)[0], "w_out": f(w_out)[0], "pst": pst, "g1": f(norm1_g), "g2": f(norm2_g),
        "convw": f(conv_w)[0], "bm": bm, "wr": np.ascontiguousarray(wr), "br": np.ascontiguousarray(br),
        "wg": f(w_gate)[0].reshape(NE, D, 1024), "wu": f(w_up)[0].reshape(NE, D, 1024),
        "wd": f(w_down)[0].reshape(NE, 1024, D),
    }
    in_maps = []
    for c in range(NCORES):
        m = dict(shared)
        m["xh"] = np.ascontiguousarray(xpad[TOK * c: TOK * c + NT])
        first_valid = HALO - TOK * c
        vc = np.zeros((128, NKT), np.float32)
        for ti, (s0, stp) in enumerate(KT):
            tp = s0 + stp * np.arange(128)
            vc[:, ti] = (tp >= first_valid).astype(np.float32)
        m["vcol"] = vc
        in_maps.append(m)
    return in_maps


def kernel(**inputs):
    in_maps = prep_inputs(**inputs)
    if "nc" not in _CACHE:
        _CACHE["nc"] = build_program()
    nc = _CACHE["nc"]
    res = run_bass_kernel_spmd(nc, in_maps, core_ids=list(range(NCORES)))
    out = np.concatenate([r["out"] for r in res.results], axis=0)
    return out.reshape(1, SEQ, D).astype(np.float32)
```
